# Optimizing a Trainium2 kernel written in Bass

```python
import jax, jax.numpy as jnp
from jax import lax
import numpy as np


D_MODEL = 1024
BATCH = 8
SEQ = 2048
DEPTH = 1

GLA_HEADS = 4
GLA_DK = 64
GLA_DV = 128
GLA_WIDTH = GLA_HEADS * GLA_DV
GLA_GATE_RANK = 16
GLA_GATE_NORM = 16.0
GDN_HEADS = 4
GDN_DK = 128
GDN_DV = 128
GDN_WIDTH = GDN_HEADS * GDN_DV
CONV_W = 4
CHUNK = 64
N_GROUPS = 8
EXPERTS_PER_GROUP = 8
N_EXPERTS = N_GROUPS * EXPERTS_PER_GROUP
TOP_K = 2
D_EXPERT = 512
DISPATCH_BLOCK = 128
LN_EPS = 1e-5
RMS_EPS = 1e-6
ALPHA_DN = (2.0 * DEPTH) ** 0.25
BETA_DN = (8.0 * DEPTH) ** -0.25

_SEG_SIZES = (GLA_HEADS * GLA_DK, GLA_HEADS * GLA_DK, GLA_WIDTH, GLA_WIDTH, GLA_GATE_RANK,
              GDN_HEADS * GDN_DK, GDN_HEADS * GDN_DK, GDN_WIDTH, GDN_WIDTH, GDN_HEADS, GDN_HEADS)
IN_PROJ_DIM = sum(_SEG_SIZES)
CONV_CH = 2 * GDN_HEADS * GDN_DK + GDN_WIDTH

kernel_name = 'hybrid_gla_gdn_hmoe_deepnorm'


def _layer_norm(x, g, b):
    xf = x.astype(jnp.float32)
    mu = jnp.mean(xf, axis=-1, keepdims=True)
    var = jnp.mean(jnp.square(xf - mu), axis=-1, keepdims=True)
    return ((xf - mu) * lax.rsqrt(var + LN_EPS) * g.astype(jnp.float32) + b.astype(jnp.float32)).astype(x.dtype)


def _rms_norm(x, g):
    return x * lax.rsqrt(jnp.mean(jnp.square(x), axis=-1, keepdims=True) + RMS_EPS) * g.astype(jnp.float32)


def _l2_norm(x):
    return x * lax.rsqrt(jnp.sum(jnp.square(x), axis=-1, keepdims=True) + RMS_EPS)


def _to_chunks(t, heads):
    b, s, w = t.shape
    return t.reshape(b, s // CHUNK, CHUNK, heads, w // heads).transpose(1, 0, 3, 2, 4)


def _to_chunks_scalar(t):
    b, s, h = t.shape
    return t.reshape(b, s // CHUNK, CHUNK, h).transpose(1, 0, 3, 2)


def _from_chunks(t):
    nc, b, h, c, d = t.shape
    return t.transpose(1, 0, 3, 2, 4).reshape(b, nc * c, h, d)


def _causal_conv(x, w):
    c = x.shape[-1]
    return lax.conv_general_dilated(x, w[:, None, :], window_strides=(1,), padding=[(CONV_W - 1, 0)],
                                    dimension_numbers=('NWC', 'WIO', 'NWC'), feature_group_count=c)


def _gla_chunked(q, k, v, g):
    nc, bsz, nh, c, dk = q.shape
    dv = v.shape[-1]
    causal = jnp.tril(jnp.ones((c, c), dtype=bool))

    def step(state, inp):
        qc, kc, vc, gc = inp
        b = jnp.cumsum(gc, axis=-2)
        rel = jnp.exp(jnp.where(causal[:, :, None], b[..., :, None, :] - b[..., None, :, :], -jnp.inf))
        attn = jnp.einsum('bhid,bhjd,bhijd->bhij', qc, kc, rel)
        o = jnp.einsum('bhid,bhde->bhie', qc * jnp.exp(b), state) + jnp.einsum('bhij,bhje->bhie', attn, vc)
        b_last = b[..., -1:, :]
        state = jnp.exp(b_last[..., 0, :])[..., None] * state + jnp.einsum('bhjd,bhje->bhde', kc * jnp.exp(b_last - b), vc)
        return state, o

    s0 = jnp.zeros((bsz, nh, dk, dv), q.dtype)
    _, o = lax.scan(step, s0, (q, k, v, g))
    return o


def _gdn_chunked(q, k, v, beta, g):
    c = q.shape[-2]
    dv = v.shape[-1]
    causal = jnp.tril(jnp.ones((c, c), dtype=bool))
    strict = jnp.tril(jnp.ones((c, c), dtype=bool), -1)
    eye = jnp.eye(c, dtype=q.dtype)
    gam = jnp.cumsum(g, axis=-1)
    decay = jnp.exp(jnp.where(causal, gam[..., :, None] - gam[..., None, :], -jnp.inf))
    a_mat = jnp.where(strict, beta[..., :, None] * jnp.einsum('nbhid,nbhjd->nbhij', k, k) * decay, 0.0)
    rhs = jnp.concatenate([beta[..., None] * v, beta[..., None] * k * jnp.exp(gam)[..., None]], axis=-1)
    uw = lax.linalg.triangular_solve(eye + a_mat, rhs, left_side=True, lower=True, unit_diagonal=True)
    u, w = uw[..., :dv], uw[..., dv:]
    attn = jnp.einsum('nbhid,nbhjd->nbhij', q, k) * decay
    q_dec = q * jnp.exp(gam)[..., None]
    gam_last = gam[..., -1:]
    k_dec = k * jnp.exp(gam_last - gam)[..., None]
    chunk_decay = jnp.exp(gam_last[..., 0])

    def step(state, inp):
        qd, at, uc, wc, kd, dl = inp
        v_new = uc - jnp.einsum('bhid,bhde->bhie', wc, state)
        o = jnp.einsum('bhid,bhde->bhie', qd, state) + jnp.einsum('bhij,bhje->bhie', at, v_new)
        state = dl[..., None, None] * state + jnp.einsum('bhjd,bhje->bhde', kd, v_new)
        return state, o

    nc, bsz, nh, _, dk = q.shape
    s0 = jnp.zeros((bsz, nh, dk, dv), q.dtype)
    _, o = lax.scan(step, s0, (q_dec, attn, u, w, k_dec, chunk_decay))
    return o


def _mixer(h, w_in, w_gk_up, b_gk, conv_w, a_log, dt_bias, gla_norm_g, gdn_norm_g, w_out):
    f32 = jnp.float32
    bsz, seq, _ = h.shape
    proj = (h @ w_in).astype(f32)
    qa, ka, va, ra, lra, qb, kb, vb, zb, bb, ab = jnp.split(proj, np.cumsum(_SEG_SIZES)[:-1].tolist(), axis=-1)
    gk = jax.nn.log_sigmoid(lra @ w_gk_up.astype(f32) + b_gk.astype(f32)) / GLA_GATE_NORM
    o_a = _gla_chunked(_to_chunks(qa * GLA_DK ** -0.5, GLA_HEADS), _to_chunks(ka, GLA_HEADS),
                       _to_chunks(va, GLA_HEADS), _to_chunks(gk, GLA_HEADS))
    o_a = _rms_norm(_from_chunks(o_a), gla_norm_g) * jax.nn.silu(ra.reshape(bsz, seq, GLA_HEADS, GLA_DV))
    qkv = jax.nn.silu(_causal_conv(jnp.concatenate([qb, kb, vb], axis=-1), conv_w.astype(f32)))
    qb, kb, vb = jnp.split(qkv, [GDN_HEADS * GDN_DK, 2 * GDN_HEADS * GDN_DK], axis=-1)
    qb = _l2_norm(_to_chunks(qb, GDN_HEADS)) * GDN_DK ** -0.5
    kb = _l2_norm(_to_chunks(kb, GDN_HEADS))
    beta = _to_chunks_scalar(jax.nn.sigmoid(bb))
    g_b = _to_chunks_scalar(-jnp.exp(a_log.astype(f32)) * jax.nn.softplus(ab + dt_bias.astype(f32)))
    o_b = _gdn_chunked(qb, kb, _to_chunks(vb, GDN_HEADS), beta, g_b)
    o_b = _rms_norm(_from_chunks(o_b), gdn_norm_g) * jax.nn.silu(zb.reshape(bsz, seq, GDN_HEADS, GDN_DV))
    mixed = jnp.concatenate([o_a.reshape(bsz, seq, GLA_WIDTH), o_b.reshape(bsz, seq, GDN_WIDTH)], axis=-1)
    return (mixed @ w_out.astype(f32)).astype(h.dtype)


def _moe(h, w_router_group, b_router_group, w_router_expert, b_router_expert, w_gate, w_up, w_down):
    f32 = jnp.float32
    bsz, seq, d = h.shape
    n = bsz * seq
    xt = h.reshape(n, d)
    xf = xt.astype(f32)
    group_prob = jax.nn.softmax(xf @ w_router_group.astype(f32) + b_router_group.astype(f32), axis=-1)
    p_group, g_sel = lax.top_k(group_prob, 1)
    g_sel = g_sel[:, 0]
    e_logits = (xf @ w_router_expert.astype(f32) + b_router_expert.astype(f32)).reshape(n, N_GROUPS, EXPERTS_PER_GROUP)
    e_logits = e_logits[jnp.arange(n), g_sel]
    top_logit, top_e = lax.top_k(e_logits, TOP_K)
    gate = p_group * jax.nn.softmax(top_logit, axis=-1)
    expert_id = (g_sel[:, None] * EXPERTS_PER_GROUP + top_e).reshape(-1)
    n_assign = n * TOP_K
    tok = jnp.arange(n_assign, dtype=jnp.int32) // TOP_K
    order = jnp.argsort(expert_id)
    e_sorted = expert_id[order]
    counts = jnp.bincount(expert_id, length=N_EXPERTS)
    starts = jnp.cumsum(counts) - counts
    padded = (counts + DISPATCH_BLOCK - 1) // DISPATCH_BLOCK * DISPATCH_BLOCK
    pends = jnp.cumsum(padded)
    pstarts = pends - padded
    dest_sorted = pstarts[e_sorted] + jnp.arange(n_assign, dtype=jnp.int32) - starts[e_sorted]
    dest = jnp.zeros((n_assign,), jnp.int32).at[order].set(dest_sorted.astype(jnp.int32))
    n_blocks = -(-n_assign // DISPATCH_BLOCK) + N_EXPERTS
    buf_tok = jnp.full((n_blocks * DISPATCH_BLOCK,), n, dtype=jnp.int32).at[dest].set(tok)
    x_pad = jnp.concatenate([xt, jnp.zeros((1, d), xt.dtype)], axis=0)
    xbuf = x_pad[buf_tok].reshape(n_blocks, DISPATCH_BLOCK, d)
    blk_expert = jnp.minimum(jnp.searchsorted(pends, jnp.arange(n_blocks) * DISPATCH_BLOCK, side='right'), N_EXPERTS - 1)

    def expert_block(blk):
        xb, e = blk
        hid = jax.nn.silu(xb @ w_gate[e]) * (xb @ w_up[e])
        return hid @ w_down[e]

    ybuf = lax.map(expert_block, (xbuf, blk_expert)).reshape(n_blocks * DISPATCH_BLOCK, d)
    y_assign = ybuf[dest].reshape(n, TOP_K, d)
    y = jnp.einsum('nkd,nk->nd', y_assign, gate.astype(y_assign.dtype))
    return y.reshape(bsz, seq, d).astype(h.dtype)


def setup_inputs(seed: int = 0) -> dict:
    key = jax.random.key(seed)
    ks = jax.random.split(key, 24)
    f32 = jnp.float32
    L = DEPTH

    def nrm(k, shape, scale):
        return jax.random.normal(k, shape, f32) * scale

    x = nrm(ks[0], (BATCH, SEQ, D_MODEL), 1.0)
    w_in = nrm(ks[1], (L, D_MODEL, IN_PROJ_DIM), D_MODEL ** -0.5)
    w_gk_up = nrm(ks[2], (L, GLA_GATE_RANK, GLA_HEADS * GLA_DK), GLA_GATE_RANK ** -0.5)
    b_gk = nrm(ks[3], (L, GLA_HEADS * GLA_DK), 0.1)
    conv_w = nrm(ks[4], (L, CONV_W, CONV_CH), CONV_W ** -0.5)
    a_log = jnp.log(jax.random.uniform(ks[5], (L, GDN_HEADS), f32, minval=1.0, maxval=16.0))
    dt = jnp.exp(jax.random.uniform(ks[6], (L, GDN_HEADS), f32, minval=float(np.log(1e-3)), maxval=float(np.log(1e-1))))
    dt_bias = dt + jnp.log(-jnp.expm1(-dt))
    gla_norm_g = 1.0 + nrm(ks[7], (L, GLA_DV), 0.02)
    gdn_norm_g = 1.0 + nrm(ks[8], (L, GDN_DV), 0.02)
    w_out = nrm(ks[9], (L, GLA_WIDTH + GDN_WIDTH, D_MODEL), (GLA_WIDTH + GDN_WIDTH) ** -0.5 * BETA_DN)
    ln1_g = 1.0 + nrm(ks[10], (L, D_MODEL), 0.02)
    ln1_b = nrm(ks[11], (L, D_MODEL), 0.02)
    w_router_group = nrm(ks[12], (L, D_MODEL, N_GROUPS), D_MODEL ** -0.5)
    b_router_group = nrm(ks[13], (L, N_GROUPS), 0.01)
    w_router_expert = nrm(ks[14], (L, D_MODEL, N_EXPERTS), D_MODEL ** -0.5)
    b_router_expert = nrm(ks[15], (L, N_EXPERTS), 0.01)
    w_gate = nrm(ks[16], (L, N_EXPERTS, D_MODEL, D_EXPERT), D_MODEL ** -0.5)
    w_up = nrm(ks[17], (L, N_EXPERTS, D_MODEL, D_EXPERT), D_MODEL ** -0.5)
    w_down = nrm(ks[18], (L, N_EXPERTS, D_EXPERT, D_MODEL), D_EXPERT ** -0.5 * BETA_DN)
    ln2_g = 1.0 + nrm(ks[19], (L, D_MODEL), 0.02)
    ln2_b = nrm(ks[20], (L, D_MODEL), 0.02)
    return {'x': x, 'w_in': w_in, 'w_gk_up': w_gk_up, 'b_gk': b_gk, 'conv_w': conv_w,
            'a_log': a_log, 'dt_bias': dt_bias, 'gla_norm_g': gla_norm_g, 'gdn_norm_g': gdn_norm_g,
            'w_out': w_out, 'ln1_g': ln1_g, 'ln1_b': ln1_b,
            'w_router_group': w_router_group, 'b_router_group': b_router_group,
            'w_router_expert': w_router_expert, 'b_router_expert': b_router_expert,
            'w_gate': w_gate, 'w_up': w_up, 'w_down': w_down, 'ln2_g': ln2_g, 'ln2_b': ln2_b}


def reference(x, w_in, w_gk_up, b_gk, conv_w, a_log, dt_bias, gla_norm_g, gdn_norm_g, w_out,
              ln1_g, ln1_b, w_router_group, b_router_group, w_router_expert, b_router_expert,
              w_gate, w_up, w_down, ln2_g, ln2_b):
    for l in range(DEPTH):
        y = _mixer(x, w_in[l], w_gk_up[l], b_gk[l], conv_w[l], a_log[l], dt_bias[l],
                   gla_norm_g[l], gdn_norm_g[l], w_out[l])
        x = _layer_norm(ALPHA_DN * x + y, ln1_g[l], ln1_b[l])
        y = _moe(x, w_router_group[l], b_router_group[l], w_router_expert[l], b_router_expert[l],
                 w_gate[l], w_up[l], w_down[l])
        x = _layer_norm(ALPHA_DN * x + y, ln2_g[l], ln2_b[l])
    return x
```

```python
import numpy as np
from contextlib import ExitStack
import concourse.bass as bass
import concourse.mybir as mybir
from concourse.bass_utils import run_bass_kernel_spmd

F32 = mybir.dt.float32
BF16 = mybir.dt.bfloat16
I32 = mybir.dt.int32
AF = mybir.ActivationFunctionType
ALU = mybir.AluOpType
AX = mybir.AxisListType

D = 1024
S = 2048
NT = S // 128
NCORES = 8
INP = 3608
ALPHA = 2.0 ** 0.25
NEXP = 64
DEXP = 512
CAP = 128
YP = S + 1
NEG = -30000.0
SAME_ENGINE_SYNC = True
import os
CUT = int(os.environ.get("CUT", "0"))

O_QA, O_KA, O_VA, O_RA, O_LRA, O_QB, O_KB, O_VB, O_ZB, O_BB = 0, 256, 512, 1024, 1536, 1552, 2064, 2576, 3088, 3600

C_ID, C_ONE, C_MI, C_NM, C_ST, C_SL, C_U, C_IO, C_SC, C_TOK = 0, 128, 256, 384, 512, 640, 768, 896, 960, 1088
CW = 1089
P_BGK, P_CONV, P_ALOG, P_DTB, P_RB = 0, 2, 50, 54, 58
PW = 130


class Buf:
    __slots__ = ("w", "r")

    def __init__(self):
        self.w = None
        self.r = {}


class Tl:
    def __init__(self, t):
        self.t = t
        self.b = Buf()

    def __getitem__(self, k):
        return self.t[k]


class Op:
    __slots__ = ("eng", "fn", "waits", "signal", "value", "key", "ordinal", "isdma")


class Prog:
    ENGS = ("pe", "act", "dve", "pool", "sp")

    def __init__(self, nc, es):
        self.nc = nc
        self.es = es
        self.streams = {e: [] for e in self.ENGS}
        self.sems = {}
        self.dcount = {}
        self.waited = {}
        self.ecount = {e: 0 for e in self.ENGS}
        for e in self.ENGS:
            self.sems[e] = es.enter_context(nc.semaphore("s_" + e))
        self.nt = 0
        self.rot = {}
        self.finalized = set()
        self.last_dma = {}

    def dsem(self, name):
        if name not in self.sems:
            self.sems[name] = self.es.enter_context(self.nc.semaphore("d_" + name))
            self.dcount[name] = 0
        return name

    def sb(self, shape, dt, name=None):
        self.nt += 1
        return Tl(self.es.enter_context(self.nc.sbuf_tensor("sb_" + (name or f"t{self.nt}"), list(shape), dt)))

    def ps(self, shape, dt, name=None):
        self.nt += 1
        return Tl(self.es.enter_context(self.nc.psum_tensor("ps_" + (name or f"p{self.nt}"), list(shape), dt)))

    def _dep(self, op, prod):
        if prod is None:
            return
        if prod.key == op.eng and not prod.isdma:
            if op.eng == "pe" or not SAME_ENGINE_SYNC:
                return
        k = (op.eng, prod.key)
        if self.waited.get(k, 0) >= prod.ordinal:
            return
        self.waited[k] = prod.ordinal
        prod.signal = True
        op.waits.append(prod)

    def _mk(self, eng, fn, r, w, key, isdma):
        op = Op()
        op.eng, op.fn, op.waits, op.signal, op.value, op.key, op.isdma = eng, fn, [], isdma, 0, key, isdma
        if isdma:
            self.dcount[key] += 1
            op.ordinal = self.dcount[key]
        else:
            self.ecount[eng] += 1
            op.ordinal = self.ecount[eng]
        if isdma:
            self._dep(op, self.last_dma.get(key))
            self.last_dma[key] = op
        for t in r:
            self._dep(op, t.b.w)
        for t in w:
            self._dep(op, t.b.w)
            for p in list(t.b.r.values()):
                self._dep(op, p)
        for t in r:
            t.b.r[key] = op
        for t in w:
            t.b.w = op
            t.b.r = {}
        self.streams[eng].append(op)
        return op

    def op(self, eng, fn, r=(), w=()):
        return self._mk(eng, fn, r, w, eng, False)

    def dma(self, eng, fn, sem, r=(), w=(), n=1):
        st = self.rot.setdefault(sem, [0])
        key = self.dsem(f"{sem}{st[0] % n}")
        st[0] += 1
        return self._mk(eng, fn, r, w, key, True)

    def wait_all_dma(self, eng, sem):
        for key in list(self.dcount):
            if key.rstrip("0123456789") != sem or key in self.finalized:
                continue
            self.finalized.add(key)
            op = Op()
            op.eng, op.fn, op.waits, op.signal, op.value, op.key, op.isdma = eng, None, [], False, 0, eng, False
            op.ordinal = 0
            op.fn = ("wait", self.sems[key], 16 * self.dcount[key])
            self.streams[eng].append(op)

    def barrier(self):
        lasts = []
        for e in self.ENGS:
            for o in reversed(self.streams[e]):
                if isinstance(o.fn, tuple):
                    continue
                if not o.isdma:
                    lasts.append(o)
                    break
        dl = {}
        for e in self.ENGS:
            for o in self.streams[e]:
                if o.isdma:
                    dl[o.key] = o
        for e in self.ENGS:
            op = Op()
            op.eng, op.fn, op.waits, op.signal, op.value, op.key, op.isdma = e, ("nop",), [], False, 0, e, False
            op.ordinal = 0
            for p in lasts + list(dl.values()):
                if p.key == e and not p.isdma:
                    continue
                k = (e, p.key)
                if self.waited.get(k, 0) >= p.ordinal:
                    continue
                self.waited[k] = p.ordinal
                p.signal = True
                op.waits.append(p)
            self.streams[e].append(op)

    def emit(self):
        for e in self.ENGS:
            c = 0
            for o in self.streams[e]:
                if isinstance(o.fn, tuple):
                    continue
                if o.isdma:
                    o.value = 16 * o.ordinal
                elif o.signal:
                    c += 1
                    o.value = c
        engobj = {"pe": "tensor", "act": "scalar", "dve": "vector", "pool": "gpsimd", "sp": "sync"}
        with self.nc.Block() as block:
            for e in self.ENGS:
                stream = self.streams[e]
                sems = self.sems

                def body(E, stream=stream, e=e):
                    for o in stream:
                        for p in o.waits:
                            E.wait_ge(sems[p.key], p.value)
                        if isinstance(o.fn, tuple):
                            if o.fn[0] == "wait":
                                E.wait_ge(o.fn[1], o.fn[2])
                            continue
                        ins = o.fn(E)
                        if o.isdma:
                            ins.then_inc(sems[o.key], 16)
                        elif o.signal:
                            ins.then_inc(sems[e], 1)

                getattr(block, engobj[e])(body)


def make_consts():
    c = np.zeros((128, CW), np.float32)
    j = np.arange(128)[:, None]
    i = np.arange(128)[None, :]
    same = (j // 64) == (i // 64)
    c[:, C_ID:C_ID + 128] = (j == i)
    c[:, C_ONE:C_ONE + 128] = 1.0
    mi = same & (j <= i)
    c[:, C_MI:C_MI + 128] = mi
    c[:, C_NM:C_NM + 128] = np.where(mi, 0.0, NEG)
    c[:, C_ST:C_ST + 128] = same & (j < i)
    c[:, C_SL:C_SL + 128] = same & (j > i)
    c[:, C_U:C_U + 128] = (j < i)
    c[:, C_IO:C_IO + 64] = np.arange(64)[None, :]
    sc = np.ones(128, np.float32)
    sc[0] = 0.0
    sc[64] = 0.0
    c[:, C_SC:C_SC + 128] = sc[None, :]
    c[:, C_TOK] = np.arange(128)
    return c


def build(nt=NT, dbg=False, moe=True):
    nc = bass.Bass("TRN2", target_bir_lowering=False)

    def din(name, shape, dt=F32):
        return nc.dram_tensor(name, list(shape), dt, kind="ExternalInput").ap()

    xT_d = din("xT", [D, S])
    x_d = din("x", [S, D])
    win_d = din("w_in", [D, INP])
    wout_d = din("w_out", [D, D])
    wgk_d = din("w_gk", [16, 256])
    small_d = din("small", [128, PW])
    gn_d = din("gn", [128, 1024])
    ln1_d = din("ln1", [128, 2048])
    ln2_d = din("ln2", [128, 2048])
    consts_d = din("consts", [128, CW])
    wr_d = din("w_r", [D, 72])
    if moe:
        wg_d = din("w_gate", [NEXP, D, DEXP])
        wu_d = din("w_up", [NEXP, D, DEXP])
        wd_d = din("w_down", [NEXP, DEXP, D])
    out_d = nc.dram_tensor("out", [S, D], F32, kind="ExternalOutput").ap()
    if dbg:
        dbg_d = nc.dram_tensor("dbg", [S, D], F32, kind="ExternalOutput").ap()
    x1f_d = nc.dram_tensor("x1f", [S, D], F32, kind="Internal").ap()
    x1b_d = nc.dram_tensor("x1b", [S + 1, D], BF16, kind="Internal").ap()
    ysc_d = nc.dram_tensor("ysc", [NEXP * CAP + 128, D], F32, kind="Internal").ap()
    tab_d = nc.dram_tensor("tab", [NEXP * CAP + 128, 2], I32, kind="Internal").ap()

    with ExitStack() as es:
        P = Prog(nc, es)
        sb, ps = P.sb, P.ps

        big = sb([128, 8 * INP + 8 * D], BF16, "big")
        win_v = big.t[:, 0:8 * INP].rearrange("p (k c) -> p k c", k=8)
        wout_v = big.t[:, 8 * INP:8 * INP + 8 * D].rearrange("p (k c) -> p k c", k=8)
        cst = sb([128, CW], F32, "cst")
        small = sb([128, PW], F32, "small")
        gn = sb([128, 1024], F32, "gn")
        lnp = sb([128, 2048], F32, "lnp")
        wgk = sb([16, 256], F32, "wgk")
        wr = sb([128, 8, 72], F32, "wr")
        ident_bf = sb([128, 128], BF16, "ident_bf")
        ones_bf = sb([128, 128], BF16, "ones_bf")
        mask4_bf = sb([128, 512], BF16, "mask4")
        u_bf = sb([128, 128], BF16, "u_bf")
        negb = sb([128, 2], F32, "negb")
        negA = sb([128, 4], F32, "negA")
        epsT = sb([128, 2], F32, "epsT")
        hm = sb([128, 2], F32, "hm")
        P.op("pool", lambda E: E.memset(epsT.t[:, 0:1], 1e-6), w=[epsT])
        P.op("pool", lambda E: E.memset(epsT.t[:, 1:2], 1e-5), w=[epsT])

        ident = cst.t[:, C_ID:C_ID + 128]
        ones = cst.t[:, C_ONE:C_ONE + 128]
        m_incl = cst.t[:, C_MI:C_MI + 128]
        negmask = cst.t[:, C_NM:C_NM + 128]
        strict01 = cst.t[:, C_ST:C_ST + 128]
        strictL = cst.t[:, C_SL:C_SL + 128]
        scanmask = cst.t[:, C_SC:C_SC + 128]

        pbanks = [ps([128, 512], F32, f"pb{i}") for i in range(6)]
        tbanks = [ps([128, 1024], BF16, f"tb{i}") for i in range(2)]
        rr = {"p": 0, "t": 0}

        def pbank():
            rr["p"] = (rr["p"] + 1) % 5
            return pbanks[rr["p"]]

        def tbank():
            rr["t"] = (rr["t"] + 1) % 2
            return tbanks[rr["t"]]

        pools = {}

        def tmp(name, shape, dt, n=2):
            if name not in pools:
                pools[name] = [[sb(shape, dt, f"{name}_{i}") for i in range(n)], 0]
            pl = pools[name]
            pl[1] = (pl[1] + 1) % len(pl[0])
            return pl[0][pl[1]]

        def DD(name, tl, ncols, t0=0, rows=128):
            if dbg != name:
                return
            od = tmp("od", [128, 512], F32, 1)
            P.op("dve", lambda E: E.tensor_copy(out=od.t[0:rows, 0:ncols], in_=tl.t[0:rows, 0:ncols]), r=[tl], w=[od])
            P.dma("sp", lambda E: E.dma_start(out=dbg_d[t0:t0 + rows, 0:ncols], in_=od.t[0:rows, 0:ncols]), "dbg", r=[od])

        P.dma("sp", lambda E: E.dma_start(out=cst.t[:], in_=consts_d), "lda", w=[cst])
        P.dma("sp", lambda E: E.dma_start(out=small.t[:], in_=small_d), "ldb", w=[small])
        P.dma("sp", lambda E: E.dma_start(out=gn.t[:], in_=gn_d), "ldc", w=[gn])
        P.dma("sp", lambda E: E.dma_start(out=lnp.t[:], in_=ln1_d), "ldd", w=[lnp])
        P.dma("sp", lambda E: E.dma_start(out=wgk.t[:], in_=wgk_d), "lde", w=[wgk])
        P.dma("sp", lambda E: E.dma_start(out=wr.t[:], in_=wr_d.rearrange("(k p) c -> p k c", p=128)), "ldf", w=[wr])
        class DT0:
            def __init__(self):
                self.b = Buf()
        win_c = [DT0() for _ in range(8)]
        wout_c = [DT0() for _ in range(8)]
        for k in range(8):
            P.dma("pool", lambda E, k=k: E.dma_start(out=win_v[:, k, :], in_=win_d[k * 128:(k + 1) * 128, :]), "ldw", w=[win_c[k]], n=8)
        for k in range(8):
            P.dma("pool", lambda E, k=k: E.dma_start(out=wout_v[:, k, :], in_=wout_d[k * 128:(k + 1) * 128, :]), "ldw", w=[wout_c[k]], n=8)

        P.op("dve", lambda E: E.tensor_copy(out=ident_bf.t[:], in_=ident), r=[cst], w=[ident_bf])
        P.op("dve", lambda E: E.tensor_copy(out=ones_bf.t[:], in_=ones), r=[cst], w=[ones_bf])
        P.op("dve", lambda E: E.tensor_copy(out=u_bf.t[:], in_=cst.t[:, C_U:C_U + 128]), r=[cst], w=[u_bf])
        for h in range(4):
            P.op("dve", lambda E, h=h: E.tensor_copy(out=mask4_bf.t[:, h * 128:(h + 1) * 128], in_=m_incl), r=[cst], w=[mask4_bf])
        P.op("dve", lambda E: E.tensor_scalar(out=negb.t[:], in0=small.t[:, P_BGK:P_BGK + 2], scalar1=-1.0, scalar2=None, op0=ALU.mult), r=[small], w=[negb])
        P.op("act", lambda E: E.activation(out=negA.t[:], in_=small.t[:, P_ALOG:P_ALOG + 4], func=AF.Exp), r=[small], w=[negA])
        P.op("dve", lambda E: E.tensor_scalar(out=negA.t[:], in0=negA.t[:], scalar1=-1.0, scalar2=None, op0=ALU.mult), r=[negA], w=[negA])

        P.op("dve", lambda E: E.tensor_scalar(out=hm.t[:, 0:1], in0=cst.t[:, C_TOK:C_TOK + 1], scalar1=64.0, scalar2=None, op0=ALU.is_lt), r=[cst], w=[hm])
        P.op("dve", lambda E: E.tensor_scalar(out=hm.t[:, 1:2], in0=cst.t[:, C_TOK:C_TOK + 1], scalar1=64.0, scalar2=None, op0=ALU.is_ge), r=[cst], w=[hm])
        class DT:
            def __init__(self):
                self.b = Buf()
        x1f_T, x1b_T, tab_T, ysc_T = DT(), DT(), DT(), DT()
        gates = sb([128, 2 * NT], F32, "gates")
        dest_i = sb([128, 2 * NT], I32, "dest_i")
        Macc = sb([128, 64], BF16, "Macc")
        P.op("pool", lambda E: E.memset(Macc.t[:], 0.0), w=[Macc])
        if moe:
            tabinit = sb([128, 130], I32, "tabinit")
            P.op("pool", lambda E: E.memset(tabinit.t[:], S), w=[tabinit])
            P.dma("sp", lambda E: E.dma_start(out=tab_d.rearrange("(n p) c -> p n c", p=128), in_=tabinit.t[:].rearrange("p (n c) -> p n c", c=2)), "tabi", r=[tabinit], w=[tab_T])
        S_gla = [sb([128, 256], F32, f"S_gla{p}") for p in range(2)]
        S_gla_bf = [[sb([128, 256], BF16, f"S_gla_bf{p}_{i}") for i in range(3)] for p in range(2)]
        S_gdn = [sb([128, 128], F32, f"S_gdn{h}") for h in range(4)]
        S_gdn_bf = [[sb([128, 128], BF16, f"S_gdn_bf{h}_{i}") for i in range(3)] for h in range(4)]
        cin = [sb([128, 131], F32, f"cin{c}") for c in range(12)]
        qpad = [sb([128, 256], BF16, f"qpad{h}") for h in range(4)]
        vnews = [sb([128, 512], BF16, f"vnew{i}") for i in range(2)]
        kpad = [sb([128, 256], BF16, f"kpad{h}") for h in range(4)]
        qdpad = [sb([128, 256], BF16, f"qdpad{h}") for h in range(4)]
        for t in S_gla + S_gdn + cin:
            P.op("pool", lambda E, t=t: E.memset(t.t[:], 0.0), w=[t])
        for t in [x for l in S_gla_bf + S_gdn_bf for x in l] + qpad + kpad + qdpad + vnews:
            P.op("pool", lambda E, t=t: E.memset(t.t[:], 0.0), w=[t])

        if moe:
            for hlf in range(2):
                P.dma("sp", lambda E, hlf=hlf: E.dma_start(out=x1b_d[S:S + 1, hlf * 512:(hlf + 1) * 512], in_=vnews[0].t[0:1, :]), "zr", r=[vnews[0]], w=[x1b_T])

        def padview(t, rows=slice(0, 128)):
            return t.t[rows, :].rearrange("p (c x) -> p c x", c=2)[:, :, 64:128]

        def v2(ap):
            return ap.rearrange("p (c x) -> p c x", c=2)

        xT_tiles = {}

        def load_xT(tt):
            t0 = tt * 128
            xTt = tmp("xTt", [128, 8, 128], BF16, 2)
            P.dma("pool", lambda E: E.dma_start(out=xTt.t[:], in_=xT_d[:, t0:t0 + 128].rearrange("(k p) t -> p k t", p=128)), "ldx", w=[xTt], n=2)
            xT_tiles[tt] = xTt

        def gdn_proj_steps(xTs):
            steps = []
            for kind, col in (("q", O_QB), ("k", O_KB), ("v", O_VB)):
                for h in range(4):
                    cb = cin[{"q": 0, "k": 4, "v": 8}[kind] + h]
                    c0 = col + h * 128

                    def step(cb=cb, c0=c0):
                        pb = pbank()
                        for k in range(8):
                            P.op("pe", lambda E, k=k: E.matmul(pb.t[:, 0:128], lhsT=win_v[:, k, c0:c0 + 128], rhs=xTs.t[:, k, :], start=(k == 0), stop=(k == 7)), r=[win_c[k], xTs], w=[pb])
                        P.op("act", lambda E: E.copy(out=cb.t[:, 3:131], in_=pb.t[:, 0:128]), r=[pb], w=[cb])
                    steps.append(step)
            return steps

        def tile_body(tt, prev_route=None):
            t0 = tt * 128
            xTt = xT_tiles.pop(tt)
            if tt + 1 < nt:
                load_xT(tt + 1)
            xtok = tmp("xtok", [128, D], F32, 1)
            P.dma("sp", lambda E, xtok=xtok, t0=t0: E.dma_start(out=xtok.t[:], in_=x_d[t0:t0 + 128, :]), "ldx2", w=[xtok])

            def fm_proj(pb, col0, m, off):
                for k in range(8):
                    P.op("pe", lambda E, k=k: E.matmul(pb.t[0:m, off:off + 128], lhsT=win_v[:, k, col0:col0 + m], rhs=xTt.t[:, k, :], start=(k == 0), stop=(k == 7)), r=[win_c[k], xTt], w=[pb])

            def tm_proj(pb, col0, n):
                for k in range(8):
                    P.op("pe", lambda E, k=k: E.matmul(pb.t[:, 0:n], lhsT=xTt.t[:, k, :], rhs=win_v[:, k, col0:col0 + n], start=(k == 0), stop=(k == 7)), r=[win_c[k], xTt], w=[pb])

            if tt == 0:
                for st_ in gdn_proj_steps(xTt):
                    st_()
            hoist = gdn_proj_steps(xT_tiles[tt + 1]) if tt + 1 < nt else []

            pb_s = pbank()
            tm_proj(pb_s, O_BB, 8)
            sc1 = tmp("sc1", [128, 8], F32)
            P.op("act", lambda E: E.activation(out=sc1.t[:, 0:4], in_=pb_s.t[:, 0:4], func=AF.Exp, scale=-1.0), r=[pb_s], w=[sc1])
            P.op("dve", lambda E: E.tensor_tensor(out=sc1.t[:, 4:8], in0=pb_s.t[:, 4:8], in1=small.t[:, P_DTB:P_DTB + 4], op=ALU.add), r=[pb_s, small], w=[sc1])
            beta = tmp("beta", [128, 4], F32)
            nbeta = tmp("nbeta", [128, 4], F32)
            P.op("dve", lambda E: E.tensor_scalar(out=sc1.t[:, 0:4], in0=sc1.t[:, 0:4], scalar1=1.0, scalar2=None, op0=ALU.add), r=[sc1], w=[sc1])
            P.op("dve", lambda E: E.reciprocal(out=beta.t[:], in_=sc1.t[:, 0:4]), r=[sc1], w=[beta])
            P.op("dve", lambda E: E.tensor_scalar(out=nbeta.t[:], in0=beta.t[:], scalar1=-1.0, scalar2=None, op0=ALU.mult), r=[beta], w=[nbeta])
            sc2 = tmp("sc2", [128, 4], F32)
            P.op("act", lambda E: E.activation(out=sc2.t[:], in_=sc1.t[:, 4:8], func=AF.Exp), r=[sc1], w=[sc2])
            P.op("act", lambda E: E.activation(out=sc2.t[:], in_=sc2.t[:], func=AF.Ln, bias=1.0), r=[sc2], w=[sc2])
            gtk = tmp("gtk", [128, 4], F32)
            P.op("dve", lambda E: E.tensor_tensor(out=gtk.t[:], in0=sc2.t[:], in1=negA.t[:], op=ALU.mult), r=[sc2, negA], w=[gtk])
            va_tok = tmp("va_tok", [128, 512], BF16)
            pb = pbank()
            tm_proj(pb, O_VA, 512)
            P.op("act", lambda E, pb=pb: E.copy(out=va_tok.t[:], in_=pb.t[:]), r=[pb], w=[va_tok])
            DD("va", va_tok, 512, t0)
            gate = tmp("gate", [128, 1024], F32, 1)
            for gi, col in enumerate((O_RA, O_ZB)):
                pb = pbank()
                tm_proj(pb, col, 512)
                P.op("act", lambda E, pb=pb, gi=gi: E.activation(out=gate.t[:, gi * 512:(gi + 1) * 512], in_=pb.t[:], func=AF.Silu), r=[pb], w=[gate])
            P.op("pool", lambda E: E.tensor_tensor(out=gate.t[:], in0=gate.t[:], in1=gn.t[:], op=ALU.mult), r=[gate, gn], w=[gate])

            pb_l = pbank()
            fm_proj(pb_l, O_LRA, 16, 0)
            lraT = tmp("lraT", [16, 128], F32)
            P.op("act", lambda E: E.copy(out=lraT.t[:], in_=pb_l.t[0:16, 0:128]), r=[pb_l], w=[lraT])
            pb_qk = pbanks[5]
            for i, col in enumerate((O_QA, O_QA + 128, O_KA, O_KA + 128)):
                fm_proj(pb_qk, col, 128, i * 128)
            qn = tmp("qn", [128, 512], BF16)
            vT = tmp("vT", [128, 512], BF16)
            kn = tmp("kn", [128, 512], BF16)
            rnb = tmp("rn", [128, 512], F32, 1)
            cvs = {"q": tmp("cv", [128, 512], F32, 2), "k": tmp("cv", [128, 512], F32, 2), "v": rnb}
            for kind in ("q", "k", "v"):
                cv = cvs[kind]
                for h in range(4):
                    ci = {"q": 0, "k": 4, "v": 8}[kind] + h
                    cb = cin[ci]
                    o = cv.t[:, h * 128:(h + 1) * 128]
                    wc = P_CONV + ci * 4
                    P.op("dve", lambda E, o=o, cb=cb, wc=wc: E.tensor_scalar(out=o, in0=cb.t[:, 0:128], scalar1=small.t[:, wc:wc + 1], scalar2=None, op0=ALU.mult), r=[cb, small], w=[cv])
                    for i in (1, 2, 3):
                        P.op("dve", lambda E, o=o, cb=cb, wc=wc, i=i: E.scalar_tensor_tensor(out=o, in0=cb.t[:, i:i + 128], scalar=small.t[:, wc + i:wc + i + 1], in1=o, op0=ALU.mult, op1=ALU.add), r=[cb, small, cv], w=[cv])
                    P.op("pool", lambda E, cb=cb: E.tensor_copy(out=cb.t[:, 0:3], in_=cb.t[:, 128:131]), r=[cb], w=[cb])
            P.op("act", lambda E: E.activation(out=cvs["q"].t[:], in_=cvs["q"].t[:], func=AF.Silu), r=[cvs["q"]], w=[cvs["q"]])
            P.op("act", lambda E: E.activation(out=cvs["k"].t[:], in_=cvs["k"].t[:], func=AF.Silu), r=[cvs["k"]], w=[cvs["k"]])
            P.op("act", lambda E: E.activation(out=vT.t[:], in_=cvs["v"].t[:], func=AF.Silu), r=[cvs["v"]], w=[vT])
            pssd = {}
            for kind in ("q", "k"):
                cv = cvs[kind]
                sq = tmp("sq", [128, 512], BF16, 1)
                P.op("pool", lambda E, cv=cv, sq=sq: E.tensor_tensor(out=sq.t[:], in0=cv.t[:], in1=cv.t[:], op=ALU.mult), r=[cv], w=[sq])
                pss = pbank()
                P.op("pe", lambda E, pss=pss, sq=sq: E.matmul(pss.t[:], lhsT=ones_bf.t[:], rhs=sq.t[:], start=True, stop=True), r=[ones_bf, sq], w=[pss])
                pssd[kind] = pss
            for kind in ("q", "k"):
                cv = cvs[kind]
                pss = pssd[kind]
                rn = rnb
                P.op("act", lambda E, pss=pss, rn=rn: E.activation(out=rn.t[:], in_=pss.t[:], func=AF.Ln, bias=epsT.t[:, 0:1]), r=[pss, epsT], w=[rn])
                P.op("act", lambda E, rn=rn: E.activation(out=rn.t[:], in_=rn.t[:], func=AF.Exp, scale=-0.5), r=[rn], w=[rn])
                if kind == "q":
                    P.op("dve", lambda E, cv=cv, rn=rn: E.scalar_tensor_tensor(out=qn.t[:], in0=cv.t[:], scalar=128.0 ** -0.5, in1=rn.t[:], op0=ALU.mult, op1=ALU.mult), r=[cv, rn], w=[qn])
                else:
                    P.op("dve", lambda E, cv=cv, rn=rn: E.tensor_tensor(out=kn.t[:], in0=cv.t[:], in1=rn.t[:], op=ALU.mult), r=[cv, rn], w=[kn])
                    for h in range(4):
                        P.op("pool", lambda E, h=h: E.tensor_copy(out=padview(kpad[h]), in_=v2(kn.t[:, h * 128:(h + 1) * 128])), r=[kn], w=[kpad[h]])
            k_tok = tmp("k_tok", [128, 512], BF16)
            v_tok = tmp("v_tok", [128, 512], BF16)
            kdec = tmp("kdec", [128, 512], BF16)
            tb = tbank()
            for h in range(4):
                P.op("pe", lambda E, tb=tb, h=h: E.transpose(tb.t[:, h * 128:(h + 1) * 128], kn.t[:, h * 128:(h + 1) * 128], ident_bf.t[:]), r=[kn, ident_bf], w=[tb])
            P.op("act", lambda E, tb=tb: E.copy(out=k_tok.t[:], in_=tb.t[:, 0:512]), r=[tb], w=[k_tok])
            tb2 = tbank()
            for h in range(4):
                P.op("pe", lambda E, tb2=tb2, h=h: E.transpose(tb2.t[:, h * 128:(h + 1) * 128], vT.t[:, h * 128:(h + 1) * 128], ident_bf.t[:]), r=[vT, ident_bf], w=[tb2])
            P.op("act", lambda E, tb2=tb2: E.copy(out=v_tok.t[:], in_=tb2.t[:, 0:512]), r=[tb2], w=[v_tok])

            pb_g = pbank()
            P.op("pe", lambda E: E.matmul(pb_g.t[:, 0:4], lhsT=m_incl, rhs=gtk.t[:], start=True, stop=True), r=[cst, gtk], w=[pb_g])
            P.op("pe", lambda E: E.matmul(pb_g.t[:, 4:8], lhsT=strictL, rhs=gtk.t[:], start=True, stop=True), r=[cst, gtk], w=[pb_g])
            gg = tmp("gg", [128, 8], F32)
            P.op("dve", lambda E: E.tensor_copy(out=gg.t[:], in_=pb_g.t[:, 0:8]), r=[pb_g], w=[gg])
            eg = tmp("eg", [128, 8], F32)
            P.op("act", lambda E: E.activation(out=eg.t[:], in_=gg.t[:], func=AF.Exp), r=[gg], w=[eg])
            neg_eg = tmp("neg_eg", [128, 4], F32)
            P.op("dve", lambda E: E.tensor_scalar(out=neg_eg.t[:], in0=eg.t[:, 0:4], scalar1=-1.0, scalar2=None, op0=ALU.mult), r=[eg], w=[neg_eg])
            D4 = tmp("D4", [128, 512], F32, 1)
            for h in range(4):
                P.op("act", lambda E, h=h: E.activation(out=D4.t[:, h * 128:(h + 1) * 128], in_=ident, func=AF.Copy, scale=gg.t[:, h:h + 1]), r=[cst, gg], w=[D4])
            pb_G = pbank()
            P.op("pe", lambda E: E.matmul(pb_G.t[:], lhsT=ones, rhs=D4.t[:], start=True, stop=True), r=[cst, D4], w=[pb_G])
            Grow = tmp("Grow", [128, 512], F32, 1)
            P.op("act", lambda E: E.copy(out=Grow.t[:], in_=pb_G.t[:]), r=[pb_G], w=[Grow])
            eG = tmp("eG", [128, 512], F32, 1)
            P.op("act", lambda E: E.activation(out=eG.t[:], in_=pb_G.t[:], func=AF.Exp), r=[pb_G], w=[eG])

            if CUT == 1:
                return
            mixed = tmp("mixed", [128, 1024], BF16, 1)

            def norm_gate(gi, po):
                sq2 = tmp("sq2", [128, 512], F32, 1)
                P.op("act", lambda E: E.activation(out=sq2.t[:], in_=po.t[:], func=AF.Square), r=[po], w=[sq2])
                ssq = tmp("ssq", [128, 4], F32)
                P.op("dve", lambda E: E.tensor_reduce(out=ssq.t[:], in_=sq2.t[:].rearrange("p (h e) -> p h e", h=4), axis=AX.X, op=ALU.add), r=[sq2], w=[ssq])
                P.op("act", lambda E: E.activation(out=ssq.t[:], in_=ssq.t[:], func=AF.Ln, scale=1.0 / 128.0, bias=epsT.t[:, 0:1]), r=[ssq, epsT], w=[ssq])
                P.op("act", lambda E: E.activation(out=ssq.t[:], in_=ssq.t[:], func=AF.Exp, scale=-0.5), r=[ssq], w=[ssq])
                for h in range(4):
                    hs = slice(h * 128, (h + 1) * 128)
                    gs = slice(gi * 512 + h * 128, gi * 512 + (h + 1) * 128)
                    P.op("dve", lambda E, hs=hs, gs=gs, h=h: E.scalar_tensor_tensor(out=mixed.t[:, gs], in0=po.t[:, hs], scalar=ssq.t[:, h:h + 1], in1=gate.t[:, gs], op0=ALU.mult, op1=ALU.mult), r=[po, ssq, gate], w=[mixed])

            if CUT == 2:
                return
            pb_gk = pbank()
            for p in range(2):
                P.op("pe", lambda E, p=p: E.matmul(pb_gk.t[:, p * 128:(p + 1) * 128], lhsT=wgk.t[:, p * 128:(p + 1) * 128], rhs=lraT.t[:], start=True, stop=True), r=[wgk, lraT], w=[pb_gk])
            khat_tok = tmp("khat_tok", [128, 256], BF16)
            ktil = []
            ktms = []
            qtil = []
            eb = []
            for p in range(2):
                l1 = tmp("l1", [128, 128], F32)
                P.op("act", lambda E, p=p, l1=l1: E.activation(out=l1.t[:], in_=pb_gk.t[:, p * 128:(p + 1) * 128], func=AF.Exp, scale=-1.0, bias=negb.t[:, p:p + 1]), r=[pb_gk, negb], w=[l1])
                P.op("act", lambda E, l1=l1: E.activation(out=l1.t[:], in_=l1.t[:], func=AF.Ln, bias=1.0), r=[l1], w=[l1])
                cs = tmp("cs", [128, 128], F32)
                P.op("dve", lambda E, l1=l1, cs=cs: E.tensor_tensor_scan(out=cs.t[:], data0=scanmask, data1=l1.t[:], initial=0.0, op0=ALU.mult, op1=ALU.add), r=[l1, cst], w=[cs])
                ebT = tmp("ebT", [128, 128], F32, 2)
                P.op("act", lambda E, cs=cs, ebT=ebT: E.activation(out=ebT.t[:], in_=cs.t[:], func=AF.Exp, scale=-1.0 / 16.0), r=[cs], w=[ebT])
                enbT = tmp("enbT", [128, 128], F32)
                P.op("act", lambda E, cs=cs, enbT=enbT: E.activation(out=enbT.t[:], in_=cs.t[:], func=AF.Exp, scale=1.0 / 16.0), r=[cs], w=[enbT])
                sfx = tmp("sfx", [128, 128], F32)
                for c in range(2):
                    P.op("dve", lambda E, cs=cs, sfx=sfx, c=c: E.tensor_scalar(out=sfx.t[:, c * 64:(c + 1) * 64], in0=cs.t[:, c * 64:(c + 1) * 64], scalar1=cs.t[:, c * 64 + 63:c * 64 + 64], scalar2=None, op0=ALU.subtract), r=[cs], w=[sfx])
                P.op("act", lambda E, sfx=sfx: E.activation(out=sfx.t[:], in_=sfx.t[:], func=AF.Exp, scale=1.0 / 16.0), r=[sfx], w=[sfx])
                qt = tmp("qtil", [128, 128], BF16, 2)
                P.op("dve", lambda E, p=p, ebT=ebT, qt=qt: E.scalar_tensor_tensor(out=qt.t[:], in0=pb_qk.t[:, p * 128:(p + 1) * 128], scalar=0.125, in1=ebT.t[:], op0=ALU.mult, op1=ALU.mult), r=[pb_qk, ebT], w=[qt])
                for hp in range(2):
                    P.op("dve", lambda E, p=p, qt=qt, hp=hp: E.tensor_scalar(out=padview(qpad[2 * p + hp]), in0=v2(qt.t[:]), scalar1=hm.t[:, hp:hp + 1], scalar2=None, op0=ALU.mult), r=[qt, hm], w=[qpad[2 * p + hp]])
                qtil.append(qt)
                kt = tmp("ktil", [128, 128], BF16, 2)
                P.op("dve", lambda E, p=p, kt=kt, enbT=enbT: E.tensor_tensor(out=kt.t[:], in0=pb_qk.t[:, 256 + p * 128:256 + (p + 1) * 128], in1=enbT.t[:], op=ALU.mult), r=[pb_qk, enbT], w=[kt])
                for hp in range(2):
                    ktm = tmp("ktm", [128, 128], BF16, 4)
                    P.op("dve", lambda E, kt=kt, hp=hp, ktm=ktm: E.tensor_scalar(out=ktm.t[:], in0=kt.t[:], scalar1=hm.t[:, hp:hp + 1], scalar2=None, op0=ALU.mult), r=[kt, hm], w=[ktm])
                    ktms.append(ktm)
                khT = tmp("khT", [128, 128], BF16)
                P.op("dve", lambda E, p=p, khT=khT, sfx=sfx: E.tensor_tensor(out=khT.t[:], in0=pb_qk.t[:, 256 + p * 128:256 + (p + 1) * 128], in1=sfx.t[:], op=ALU.mult), r=[pb_qk, sfx], w=[khT])
                tb = tbank()
                P.op("pe", lambda E, tb=tb, khT=khT: E.transpose(tb.t[:, 0:128], khT.t[:], ident_bf.t[:]), r=[khT, ident_bf], w=[tb])
                P.op("act", lambda E, tb=tb, p=p: E.copy(out=khat_tok.t[:, p * 128:(p + 1) * 128], in_=tb.t[:, 0:128]), r=[tb], w=[khat_tok])
                ktil.append(kt)
                eb.append(ebT)

            DD("qtil0", qtil[0], 128, t0); DD("ktil0", ktil[0], 128, t0); DD("eb0", eb[0], 128, t0); DD("khat", khat_tok, 256, t0)
            if CUT == 3:
                return
            pb_at = pbank()
            for h in range(4):
                p, hp = h // 2, h % 2
                rows = slice(hp * 64, hp * 64 + 64)
                P.op("pe", lambda E, h=h, p=p, rows=rows: E.matmul(pb_at.t[:, h * 128:(h + 1) * 128], lhsT=ktms[h].t[:, :], rhs=qtil[p].t[:, :], start=True, stop=True), r=[ktms[h], qtil[p]], w=[pb_at])
            at_gla = tmp("at_gla", [128, 512], BF16)
            P.op("dve", lambda E: E.tensor_tensor(out=at_gla.t[:], in0=pb_at.t[:], in1=mask4_bf.t[:], op=ALU.mult), r=[pb_at, mask4_bf], w=[at_gla])
            DD("atgla", at_gla, 512, t0)
            if CUT == 31:
                return
            si = (2 * tt) % 3
            khat_m = []
            for c in range(2):
                km = tmp("khat_m", [128, 256], BF16, 2)
                P.op("dve", lambda E, km=km, c=c: E.tensor_scalar(out=km.t[:], in0=khat_tok.t[:], scalar1=hm.t[:, c:c + 1], scalar2=None, op0=ALU.mult), r=[khat_tok, hm], w=[km])
                khat_m.append(km)
            for p in range(2):
                for c in range(2):
                    pu = pbank()
                    crow = slice(c * 64, c * 64 + 64)
                    P.op("pe", lambda E, p=p, pu=pu, c=c: E.matmul(pu.t[:, 0:256], lhsT=khat_m[c].t[:, p * 128:(p + 1) * 128], rhs=va_tok.t[:, p * 256:(p + 1) * 256], start=True, stop=True), r=[khat_m[c], va_tok], w=[pu])
                    P.op("dve", lambda E, p=p, pu=pu, c=c: E.scalar_tensor_tensor(out=S_gla[p].t[:], in0=S_gla[p].t[:], scalar=eb[p].t[:, c * 64 + 63:c * 64 + 64], in1=pu.t[:, 0:256], op0=ALU.mult, op1=ALU.add), r=[S_gla[p], eb[p], pu], w=[S_gla[p]])
                    sdst = S_gla_bf[p][(si + c + 1) % 3]
                    P.op("act", lambda E, p=p, sdst=sdst: E.copy(out=sdst.t[:], in_=S_gla[p].t[:]), r=[S_gla[p]], w=[sdst])
            if CUT == 32:
                return
            if CUT == 4:
                return
            for h in range(4):
                P.op("act", lambda E, h=h: E.activation(out=kdec.t[:, h * 128:(h + 1) * 128], in_=k_tok.t[:, h * 128:(h + 1) * 128], func=AF.Copy, scale=eg.t[:, 4 + h:5 + h]), r=[k_tok, eg], w=[kdec])

            DD("kn", kn, 512, t0); DD("qn", qn, 512, t0); DD("vtok", v_tok, 512, t0); DD("ktok", k_tok, 512, t0); DD("gg", gg, 8, t0); DD("eG", eG, 512, t0)
            if CUT == 5:
                return
            pb_kk = pbank()
            pb_qk2 = pbank()
            for h in range(4):
                P.op("pe", lambda E, h=h: E.matmul(pb_kk.t[:, h * 128:(h + 1) * 128], lhsT=kn.t[:, h * 128:(h + 1) * 128], rhs=kn.t[:, h * 128:(h + 1) * 128], start=True, stop=True), r=[kn], w=[pb_kk])
                P.op("pe", lambda E, h=h: E.matmul(pb_qk2.t[:, h * 128:(h + 1) * 128], lhsT=kn.t[:, h * 128:(h + 1) * 128], rhs=qn.t[:, h * 128:(h + 1) * 128], start=True, stop=True), r=[kn, qn], w=[pb_qk2])
            dec = tmp("dec", [128, 512], F32, 1)
            for h in range(4):
                hs = slice(h * 128, (h + 1) * 128)
                P.op("dve", lambda E, h=h, hs=hs: E.scalar_tensor_tensor(out=dec.t[:, hs], in0=Grow.t[:, hs], scalar=gg.t[:, h:h + 1], in1=negmask, op0=ALU.subtract, op1=ALU.add), r=[Grow, gg, cst], w=[dec])
            P.op("act", lambda E: E.activation(out=dec.t[:], in_=dec.t[:], func=AF.Exp), r=[dec], w=[dec])
            at_gdn = tmp("at_gdn", [128, 512], BF16)
            P.op("dve", lambda E: E.tensor_tensor(out=at_gdn.t[:], in0=pb_qk2.t[:], in1=dec.t[:], op=ALU.mult), r=[pb_qk2, dec], w=[at_gdn])
            Y = tmp("Y", [128, 512], F32, 2)
            X = tmp("X", [128, 512], F32, 2)
            Pm = tmp("Pm", [128, 512], F32, 2)
            for h in range(4):
                hs = slice(h * 128, (h + 1) * 128)
                P.op("pool", lambda E, hs=hs: E.tensor_tensor(out=dec.t[:, hs], in0=dec.t[:, hs], in1=strict01, op=ALU.mult), r=[dec, cst], w=[dec])
                P.op("dve", lambda E, h=h, hs=hs, Y=Y: E.scalar_tensor_tensor(out=Y.t[:, hs], in0=pb_kk.t[:, hs], scalar=nbeta.t[:, h:h + 1], in1=dec.t[:, hs], op0=ALU.mult, op1=ALU.mult), r=[pb_kk, nbeta, dec], w=[Y])
            for h in range(4):
                hs = slice(h * 128, (h + 1) * 128)
                P.op("dve", lambda E, h=h, hs=hs: E.tensor_tensor(out=padview(qdpad[h]), in0=v2(qn.t[:, hs]), in1=v2(eG.t[:, hs]), op=ALU.mult), r=[qn, eG], w=[qdpad[h]])
            pbx = pbank()
            for h in range(4):
                hs = slice(h * 128, (h + 1) * 128)
                P.op("pe", lambda E, hs=hs, Y=Y: E.transpose(pbx.t[:, hs], Y.t[:, hs], ident), r=[Y, cst], w=[pbx])
            P.op("act", lambda E, X=X: E.copy(out=X.t[:], in_=pbx.t[:]), r=[pbx], w=[X])
            for h in range(4):
                P.op("pool", lambda E, Pm=Pm, Y=Y, h=h: E.tensor_tensor(out=Pm.t[:, h * 128:(h + 1) * 128], in0=Y.t[:, h * 128:(h + 1) * 128], in1=ident, op=ALU.add), r=[Y, cst], w=[Pm])
            po_gla = pbank()
            for h in range(4):
                p, hp = h // 2, h % 2
                rows = slice(hp * 64, hp * 64 + 64)
                for c in range(2):
                    ssrc = S_gla_bf[p][(si + c) % 3]
                    P.op("pe", lambda E, h=h, p=p, c=c, hp=hp, rows=rows, ssrc=ssrc: E.matmul(po_gla.t[:, h * 128:(h + 1) * 128], lhsT=qpad[h].t[:, 64 + c * 64:192 + c * 64], rhs=ssrc.t[:, hp * 128:(hp + 1) * 128], start=(c == 0), stop=False), r=[qpad[h], ssrc], w=[po_gla])
                P.op("pe", lambda E, h=h: E.matmul(po_gla.t[:, h * 128:(h + 1) * 128], lhsT=at_gla.t[:, h * 128:(h + 1) * 128], rhs=va_tok.t[:, h * 128:(h + 1) * 128], start=False, stop=True), r=[at_gla, va_tok], w=[po_gla])

            if CUT == 33:
                return
            DD("ogla", po_gla, 512, t0)
            norm_gate(0, po_gla)

            TT = tmp("TT", [128, 512], BF16)
            for lvl in range(1, 6):
                pX = pbank()
                for h in range(4):
                    hs = slice(h * 128, (h + 1) * 128)
                    P.op("pe", lambda E, hs=hs, pX=pX, X=X, Y=Y: E.matmul(pX.t[:, hs], lhsT=Y.t[:, hs], rhs=X.t[:, hs], start=True, stop=True), r=[X, Y], w=[pX])
                if hoist:
                    hoist.pop(0)()
                Xn = tmp("X", [128, 512], F32, 2)
                if lvl < 5:
                    pY = pbank()
                    for h in range(4):
                        hs = slice(h * 128, (h + 1) * 128)
                        P.op("pe", lambda E, hs=hs, pY=pY, X=X, Y=Y: E.matmul(pY.t[:, hs], lhsT=X.t[:, hs], rhs=Y.t[:, hs], start=True, stop=True), r=[X, Y], w=[pY])
                    if hoist:
                        hoist.pop(0)()
                    Yn = tmp("Y", [128, 512], F32, 2)
                    P.op("act", lambda E, pY=pY, Yn=Yn: E.copy(out=Yn.t[:], in_=pY.t[:]), r=[pY], w=[Yn])
                    Y = Yn
                P.op("dve", lambda E, pX=pX, Xn=Xn: E.tensor_copy(out=Xn.t[:], in_=pX.t[:]), r=[pX], w=[Xn])
                X = Xn
                pP = pbank()
                for h in range(4):
                    hs = slice(h * 128, (h + 1) * 128)
                    P.op("pe", lambda E, hs=hs, pP=pP, X=X, Pm=Pm: E.matmul(pP.t[:, hs], lhsT=X.t[:, hs], rhs=Pm.t[:, hs], start=True, stop=True), r=[X, Pm], w=[pP])
                if hoist:
                    hoist.pop(0)()
                if lvl < 5:
                    Pn = tmp("Pm", [128, 512], F32, 2)
                    P.op("dve", lambda E, pP=pP, Pn=Pn, Pm=Pm: E.tensor_tensor(out=Pn.t[:], in0=pP.t[:], in1=Pm.t[:], op=ALU.add), r=[pP, Pm], w=[Pn])
                    Pm = Pn
                else:
                    P.op("dve", lambda E, pP=pP, Pm=Pm: E.tensor_tensor(out=TT.t[:], in0=pP.t[:], in1=Pm.t[:], op=ALU.add), r=[pP, Pm], w=[TT])

            DD("dec", dec, 512, t0); DD("TT", TT, 512, t0); DD("atgdn", at_gdn, 512, t0)
            if CUT == 6:
                return
            while hoist:
                hoist.pop(0)()
            vnew = vnews[tt % 2]
            kdec_m = []
            for c in range(2):
                km = tmp("kdec_m", [128, 512], BF16, 2)
                P.op("dve", lambda E, km=km, c=c: E.tensor_scalar(out=km.t[:], in0=kdec.t[:], scalar1=hm.t[:, c:c + 1], scalar2=None, op0=ALU.mult), r=[kdec, hm], w=[km])
                kdec_m.append(km)
            for c in range(2):
                crow = slice(c * 64, c * 64 + 64)
                pk = pbank()
                for h in range(4):
                    hs = slice(h * 128, (h + 1) * 128)
                    ssrc = S_gdn_bf[h][(si + c) % 3]
                    P.op("pe", lambda E, h=h, hs=hs, c=c, pk=pk, ssrc=ssrc: E.matmul(pk.t[:, hs], lhsT=kpad[h].t[:, 64 + c * 64:192 + c * 64], rhs=ssrc.t[:], start=True, stop=True), r=[kpad[h], ssrc], w=[pk])
                rr_ = tmp("rr", [128, 512], BF16)
                for h in range(4):
                    hs = slice(h * 128, (h + 1) * 128)
                    P.op("dve", lambda E, h=h, hs=hs, pk=pk, rr_=rr_: E.scalar_tensor_tensor(out=rr_.t[:, hs], in0=pk.t[:, hs], scalar=neg_eg.t[:, h:h + 1], in1=v_tok.t[:, hs], op0=ALU.mult, op1=ALU.add), r=[pk, neg_eg, v_tok], w=[rr_])
                pv = pbank()
                for h in range(4):
                    hs = slice(h * 128, (h + 1) * 128)
                    P.op("pe", lambda E, hs=hs, pv=pv, rr_=rr_, crow=crow: E.matmul(pv.t[:, hs], lhsT=TT.t[:, hs], rhs=rr_.t[:, hs], start=True, stop=True), r=[TT, rr_], w=[pv])
                for h in range(4):
                    hs = slice(h * 128, (h + 1) * 128)
                    P.op("act", lambda E, h=h, hs=hs, pv=pv, crow=crow: E.activation(out=vnew.t[crow, hs], in_=pv.t[crow, hs], func=AF.Copy, scale=beta.t[crow, h:h + 1]), r=[pv, beta], w=[vnew])
                for h in range(4):
                    hs = slice(h * 128, (h + 1) * 128)
                    pu = pbank()
                    P.op("pe", lambda E, hs=hs, pu=pu, c=c: E.matmul(pu.t[:, 0:128], lhsT=kdec_m[c].t[:, hs], rhs=vnew.t[:, hs], start=True, stop=True), r=[kdec_m[c], vnew], w=[pu])
                    dcol = h * 128 + c * 64 + 63
                    P.op("dve", lambda E, h=h, pu=pu, dcol=dcol: E.scalar_tensor_tensor(out=S_gdn[h].t[:], in0=S_gdn[h].t[:], scalar=eG.t[:, dcol:dcol + 1], in1=pu.t[:, 0:128], op0=ALU.mult, op1=ALU.add), r=[S_gdn[h], eG, pu], w=[S_gdn[h]])
                    sdst = S_gdn_bf[h][(si + c + 1) % 3]
                    P.op("act", lambda E, h=h, sdst=sdst: E.copy(out=sdst.t[:], in_=S_gdn[h].t[:]), r=[S_gdn[h]], w=[sdst])
            po_gdn = pbank()
            for h in range(4):
                hs = slice(h * 128, (h + 1) * 128)
                for c in range(2):
                    ssrc = S_gdn_bf[h][(si + c) % 3]
                    P.op("pe", lambda E, h=h, hs=hs, c=c, ssrc=ssrc: E.matmul(po_gdn.t[:, hs], lhsT=qdpad[h].t[:, 64 + c * 64:192 + c * 64], rhs=ssrc.t[:], start=(c == 0), stop=False), r=[qdpad[h], ssrc], w=[po_gdn])
                P.op("pe", lambda E, hs=hs: E.matmul(po_gdn.t[:, hs], lhsT=at_gdn.t[:, hs], rhs=vnew.t[:, hs], start=False, stop=True), r=[at_gdn, vnew], w=[po_gdn])

            DD("ogdn", po_gdn, 512, t0)
            norm_gate(1, po_gdn)
            if prev_route is not None:
                prev_route[0]()
            if CUT == 7:
                return
            tbm = tbank()
            for k in range(8):
                P.op("pe", lambda E, k=k, tbm=tbm: E.transpose(tbm.t[:, k * 128:(k + 1) * 128], mixed.t[:, k * 128:(k + 1) * 128], ident_bf.t[:]), r=[mixed, ident_bf], w=[tbm])
            mixT = tmp("mixT", [128, 1024], BF16, 1)
            P.op("act", lambda E, tbm=tbm: E.copy(out=mixT.t[:], in_=tbm.t[:]), r=[tbm], w=[mixT])
            z = tmp("z", [128, D], F32, 1)
            for half in range(2):
                py = pbank()
                for k in range(8):
                    P.op("pe", lambda E, k=k, py=py, half=half: E.matmul(py.t[:], lhsT=mixT.t[:, k * 128:(k + 1) * 128], rhs=wout_v[:, k, half * 512:(half + 1) * 512], start=(k == 0), stop=(k == 7)), r=[mixT, wout_c[k]], w=[py])
                P.op("dve", lambda E, py=py, half=half: E.scalar_tensor_tensor(out=z.t[:, half * 512:(half + 1) * 512], in0=xtok.t[:, half * 512:(half + 1) * 512], scalar=ALPHA, in1=py.t[:], op0=ALU.mult, op1=ALU.add), r=[xtok, py], w=[z])

            def layer_norm(z, g_ap, b_ap, gb_tl, out_tl):
                st = tmp("lnst", [128, 4], F32)
                junk = tmp("mixT", [128, 1024], BF16, 1)
                P.op("dve", lambda E: E.tensor_reduce(out=st.t[:, 0:1], in_=z.t[:], axis=AX.X, op=ALU.add), r=[z], w=[st])
                P.op("act", lambda E: E.activation(out=junk.t[:], in_=z.t[:], func=AF.Square, accum_out=st.t[:, 1:2]), r=[z], w=[junk, st])
                P.op("dve", lambda E: E.tensor_scalar(out=st.t[:, 0:2], in0=st.t[:, 0:2], scalar1=1.0 / D, scalar2=None, op0=ALU.mult), r=[st], w=[st])
                P.op("dve", lambda E: E.tensor_tensor(out=st.t[:, 2:3], in0=st.t[:, 0:1], in1=st.t[:, 0:1], op=ALU.mult), r=[st], w=[st])
                P.op("dve", lambda E: E.tensor_tensor(out=st.t[:, 2:3], in0=st.t[:, 1:2], in1=st.t[:, 2:3], op=ALU.subtract), r=[st], w=[st])
                P.op("act", lambda E: E.activation(out=st.t[:, 3:4], in_=st.t[:, 2:3], func=AF.Ln, bias=epsT.t[:, 1:2]), r=[st, epsT], w=[st])
                P.op("act", lambda E: E.activation(out=st.t[:, 3:4], in_=st.t[:, 3:4], func=AF.Exp, scale=-0.5), r=[st], w=[st])
                P.op("dve", lambda E: E.tensor_scalar(out=out_tl.t[:], in0=z.t[:], scalar1=st.t[:, 0:1], scalar2=st.t[:, 3:4], op0=ALU.subtract, op1=ALU.mult), r=[z, st], w=[out_tl])
                P.op("dve", lambda E: E.tensor_tensor(out=out_tl.t[:], in0=out_tl.t[:], in1=g_ap, op=ALU.mult), r=[out_tl, gb_tl], w=[out_tl])
                P.op("dve", lambda E: E.tensor_tensor(out=out_tl.t[:], in0=out_tl.t[:], in1=b_ap, op=ALU.add), r=[out_tl, gb_tl], w=[out_tl])

            x1 = z
            layer_norm(z, lnp.t[:, 0:D], lnp.t[:, D:2 * D], lnp, x1)
            if prev_route is not None:
                prev_route[1]()
            if dbg is True:
                P.dma("sp", lambda E, x1=x1, t0=t0: E.dma_start(out=dbg_d[t0:t0 + 128, :], in_=x1.t[:]), "dbg", r=[x1])
            if not moe:
                P.dma("sp", lambda E, x1=x1, t0=t0: E.dma_start(out=out_d[t0:t0 + 128, :], in_=x1.t[:]), "out", r=[x1])
                return
            P.dma("sp", lambda E: E.dma_start(out=x1f_d[t0:t0 + 128, :], in_=z.t[:]), "stx", r=[z, x1f_T], n=2)
            P.dma("pool", lambda E: E.dma_start(out=x1b_d[t0:t0 + 128, :], in_=z.t[:]), "stb", r=[z, x1b_T], n=2)
            x1T = xtok
            for half in range(2):
                pt = pbank()
                for j in range(4):
                    k = half * 4 + j
                    P.op("pe", lambda E, pt=pt, j=j, k=k: E.transpose(pt.t[:, j * 128:(j + 1) * 128], z.t[:, k * 128:(k + 1) * 128], ident), r=[z, cst], w=[pt])
                P.op("act", lambda E, pt=pt, half=half: E.copy(out=x1T.t[:, half * 512:(half + 1) * 512], in_=pt.t[:]), r=[pt], w=[x1T])
            plg = pbank()
            for k in range(8):
                P.op("pe", lambda E, k=k: E.matmul(plg.t[:, 0:72], lhsT=x1T.t[:, k * 128:(k + 1) * 128], rhs=wr.t[:, k, :], start=(k == 0), stop=(k == 7)), r=[x1T, wr], w=[plg])
            rt = tmp("rt", [128, 160], F32, 2)
            M1 = tmp("M1", [128, 64], F32, 2)
            M2 = tmp("M2", [128, 64], F32, 2)
            T64 = tmp("T64", [128, 64], F32, 2)
            Msb = tmp("Msb", [128, 64], BF16, 2)
            it = tmp("it", [128, 8], I32, 2)
            iota8 = cst.t[:, C_IO:C_IO + 8]
            iota64 = cst.t[:, C_IO:C_IO + 64]

            def c(a, b=None):
                return rt.t[:, a:(b if b is not None else a + 1)]

            def V(fn, er=(), ew=()):
                P.op("dve", fn, r=[rt, *er], w=[rt, *ew])

            V(lambda E: E.tensor_tensor(out=c(0, 72), in0=plg.t[:, 0:72], in1=small.t[:, P_RB:P_RB + 72], op=ALU.add), [plg, small])

            def route_a():
                V(lambda E: E.tensor_reduce(out=c(120), in_=c(0, 8), axis=AX.X, op=ALU.max))
                V(lambda E: E.tensor_scalar(out=c(72, 80), in0=c(0, 8), scalar1=c(120), scalar2=None, op0=ALU.is_equal))
                V(lambda E: E.tensor_scalar(out=c(121), in0=c(120), scalar1=-1.0, scalar2=None, op0=ALU.mult))
                P.op("act", lambda E: E.activation(out=c(140, 148), in_=c(0, 8), func=AF.Exp, bias=c(121)), r=[rt], w=[rt])
                V(lambda E: E.tensor_reduce(out=c(122), in_=c(140, 148), axis=AX.X, op=ALU.add))
                V(lambda E: E.reciprocal(out=c(123), in_=c(122)))
                V(lambda E: E.tensor_scalar(out=c(88, 96), in0=c(8, 16), scalar1=c(72), scalar2=None, op0=ALU.mult))
                for g in range(1, 8):
                    V(lambda E, g=g: E.scalar_tensor_tensor(out=c(88, 96), in0=c(8 + 8 * g, 16 + 8 * g), scalar=c(72 + g), in1=c(88, 96), op0=ALU.mult, op1=ALU.add))
                V(lambda E: E.tensor_reduce(out=c(124), in_=c(88, 96), axis=AX.X, op=ALU.max))
                V(lambda E: E.tensor_scalar(out=c(96, 104), in0=c(88, 96), scalar1=c(124), scalar2=None, op0=ALU.is_equal))
                V(lambda E: E.scalar_tensor_tensor(out=c(104, 112), in0=c(96, 104), scalar=-1e30, in1=c(88, 96), op0=ALU.mult, op1=ALU.add))
                V(lambda E: E.tensor_reduce(out=c(125), in_=c(104, 112), axis=AX.X, op=ALU.max))
                V(lambda E: E.tensor_scalar(out=c(112, 120), in0=c(104, 112), scalar1=c(125), scalar2=None, op0=ALU.is_equal))
                V(lambda E: E.tensor_tensor(out=c(126), in0=c(125), in1=c(124), op=ALU.subtract))
                P.op("act", lambda E: E.activation(out=c(126), in_=c(126), func=AF.Exp), r=[rt], w=[rt])
                V(lambda E: E.tensor_scalar(out=c(127), in0=c(126), scalar1=1.0, scalar2=None, op0=ALU.add))
                V(lambda E: E.reciprocal(out=c(127), in_=c(127)))
                V(lambda E: E.tensor_tensor(out=c(128), in0=c(126), in1=c(127), op=ALU.mult))
                P.op("dve", lambda E: E.tensor_tensor(out=gates.t[:, 2 * tt:2 * tt + 1], in0=c(123), in1=c(127), op=ALU.mult), r=[rt], w=[gates])
                P.op("dve", lambda E: E.tensor_tensor(out=gates.t[:, 2 * tt + 1:2 * tt + 2], in0=c(123), in1=c(128), op=ALU.mult), r=[rt], w=[gates])
                for src, dst in ((72, 129), (96, 130), (112, 131)):
                    V(lambda E, src=src: E.tensor_tensor(out=c(80, 88), in0=c(src, src + 8), in1=iota8, op=ALU.mult), [cst])
                    V(lambda E, dst=dst: E.tensor_reduce(out=c(dst), in_=c(80, 88), axis=AX.X, op=ALU.add))
                V(lambda E: E.scalar_tensor_tensor(out=c(132), in0=c(129), scalar=8.0, in1=c(130), op0=ALU.mult, op1=ALU.add))
                V(lambda E: E.scalar_tensor_tensor(out=c(133), in0=c(129), scalar=8.0, in1=c(131), op0=ALU.mult, op1=ALU.add))
                P.op("dve", lambda E: E.tensor_scalar(out=M1.t[:], in0=iota64, scalar1=c(132), scalar2=None, op0=ALU.is_equal), r=[rt, cst], w=[M1])
                P.op("dve", lambda E: E.tensor_scalar(out=M2.t[:], in0=iota64, scalar1=c(133), scalar2=None, op0=ALU.is_equal), r=[rt, cst], w=[M2])
                P.op("dve", lambda E: E.tensor_tensor(out=Msb.t[:], in0=M1.t[:], in1=M2.t[:], op=ALU.add), r=[M1, M2], w=[Msb])
            def route_b():
                pp = pbank()
                P.op("pe", lambda E: E.matmul(pp.t[:, 0:64], lhsT=u_bf.t[:], rhs=Msb.t[:], start=True, stop=False), r=[u_bf, Msb], w=[pp])
                P.op("pe", lambda E: E.matmul(pp.t[:, 0:64], lhsT=ones_bf.t[:], rhs=Macc.t[:], start=False, stop=True), r=[ones_bf, Macc], w=[pp])
                for Mx, dst in ((M1, 134), (M2, 135)):
                    P.op("dve", lambda E, Mx=Mx: E.tensor_tensor(out=T64.t[:], in0=Mx.t[:], in1=pp.t[:, 0:64], op=ALU.mult), r=[Mx, pp], w=[T64])
                    V(lambda E, dst=dst: E.tensor_reduce(out=c(dst), in_=T64.t[:], axis=AX.X, op=ALU.add), [T64])
                P.op("dve", lambda E: E.tensor_tensor(out=Macc.t[:], in0=Macc.t[:], in1=Msb.t[:], op=ALU.add), r=[Macc, Msb], w=[Macc])
                for eid, sl, dst, ov in ((132, 134, 136, 141), (133, 135, 137, 142)):
                    V(lambda E, eid=eid, sl=sl, dst=dst: E.scalar_tensor_tensor(out=c(dst), in0=c(eid), scalar=float(CAP), in1=c(sl), op0=ALU.mult, op1=ALU.add))
                    V(lambda E, sl=sl, ov=ov: E.tensor_scalar(out=c(ov), in0=c(sl), scalar1=CAP - 0.5, scalar2=None, op0=ALU.is_gt))
                    V(lambda E, dst=dst, ov=ov: E.scalar_tensor_tensor(out=c(ov), in0=c(dst), scalar=-float(NEXP * CAP), in1=c(ov), op0=ALU.add, op1=ALU.mult))
                    V(lambda E, dst=dst, ov=ov: E.tensor_tensor(out=c(dst), in0=c(dst), in1=c(ov), op=ALU.subtract))
                V(lambda E: E.tensor_scalar(out=c(138), in0=cst.t[:, C_TOK:C_TOK + 1], scalar1=float(t0), scalar2=None, op0=ALU.add), [cst])
                V(lambda E: E.tensor_scalar(out=c(139), in0=c(138), scalar1=float(YP), scalar2=None, op0=ALU.add))
                P.op("dve", lambda E: E.tensor_copy(out=it.t[:, 0:2], in_=c(136, 138)), r=[rt], w=[it])
                P.op("dve", lambda E: E.tensor_copy(out=dest_i.t[:, 2 * tt:2 * tt + 2], in_=c(136, 138)), r=[rt], w=[dest_i])
                for col, src in ((2, 138), (3, 138), (4, 138), (5, 139)):
                    P.op("dve", lambda E, col=col, src=src: E.tensor_copy(out=it.t[:, col:col + 1], in_=c(src)), r=[rt], w=[it])
                for k in range(2):
                    P.dma("pool", lambda E, k=k: E.indirect_dma_start(out=tab_d, out_offset=bass.IndirectOffsetOnAxis(ap=it.t[:, k:k + 1], axis=0), in_=it.t[:, 2 + 2 * k:4 + 2 * k], in_offset=None), "tabs", r=[it], w=[tab_T], n=2)
                DD("rt", rt, 160, t0)

            return (route_a, route_b)

        pending = None
        if nt > 0:
            load_xT(0)
        for tt in range(nt):
            pending = tile_body(tt, pending)
        if pending is not None:
            pending[0]()
            pending[1]()

        if moe:
            idx_sb = sb([128, NEXP, 2], I32, "idx_sb")
            P.dma("sp", lambda E: E.dma_start(out=idx_sb.t[:], in_=tab_d[0:NEXP * CAP, :].rearrange("(e s) c -> s e c", s=CAP)), "idx", w=[idx_sb, tab_T])
            wslots = [[DT() for _ in range(3)] for _ in range(3)]
            WSZ = 12288

            def load_w(e):
                sl = e % 3
                base = sl * WSZ
                first = e < 3
                ws = wslots[sl]
                wl = [((win_c + wout_c + [ws[m]]) if first else [ws[m]]) for m in range(3)]
                gv = big.t[:, base:base + 4096].rearrange("p (k f) -> p k f", k=8)
                uv = big.t[:, base + 4096:base + 8192].rearrange("p (k f) -> p k f", k=8)
                dv = big.t[:, base + 8192:base + 12288].rearrange("p (k f) -> p k f", k=4)
                P.dma("pool", lambda E: E.dma_start(out=gv, in_=wg_d[e].rearrange("(k p) f -> p k f", p=128)), "wexp", w=wl[0], n=9)
                P.dma("pool", lambda E: E.dma_start(out=uv, in_=wu_d[e].rearrange("(k p) f -> p k f", p=128)), "wexp", w=wl[1], n=9)
                P.dma("pool", lambda E: E.dma_start(out=dv, in_=wd_d[e].rearrange("(k p) f -> p k f", p=128)), "wexp", w=wl[2], n=9)
                return gv, uv, dv, ws

            class View2:
                def __init__(self, tl):
                    self.t = tl.t[:].rearrange("p k t -> p (k t)")
                    self.b = tl.b
            xg_bufs = [View2(tmp("xTt", [128, 8, 128], BF16, 2)), View2(tmp("xTt", [128, 8, 128], BF16, 2))]

            def gather(e):
                xg = xg_bufs[e % 2]
                P.dma("pool", lambda E: E.indirect_dma_start(out=xg.t[:], out_offset=None, in_=x1b_d, in_offset=bass.IndirectOffsetOnAxis(ap=idx_sb.t[:, e, 0:1], axis=0)), "gath", r=([idx_sb] if e == 0 else [idx_sb, x1b_T]), w=([xg, x1b_T] if e == 0 else [xg]), n=2)
                return xg

            def expert(e, wv, xg):
                gv, uv, dv, ws = wv
                tb = tbank()
                for k in range(8):
                    P.op("pe", lambda E, k=k: E.transpose(tb.t[:, k * 128:(k + 1) * 128], xg.t[:, k * 128:(k + 1) * 128], ident_bf.t[:]), r=[xg, ident_bf], w=[tb])
                xgT = tmp("mixT", [128, 1024], BF16, 1)
                P.op("act", lambda E: E.copy(out=xgT.t[:], in_=tb.t[:]), r=[tb], w=[xgT])
                ph = []
                for mi_, wv_ in enumerate((gv, uv)):
                    pb = pbank()
                    for k in range(8):
                        P.op("pe", lambda E, k=k, pb=pb, wv_=wv_: E.matmul(pb.t[:], lhsT=xgT.t[:, k * 128:(k + 1) * 128], rhs=wv_[:, k, :], start=(k == 0), stop=(k == 7)), r=[xgT, ws[mi_]], w=[pb])
                    ph.append(pb)
                sg = tmp("sq2", [128, 512], F32, 1)
                P.op("act", lambda E: E.activation(out=sg.t[:], in_=ph[0].t[:], func=AF.Silu), r=[ph[0]], w=[sg])
                hid = tmp("at_gla", [128, 512], BF16)
                P.op("dve", lambda E: E.tensor_tensor(out=hid.t[:], in0=sg.t[:], in1=ph[1].t[:], op=ALU.mult), r=[sg, ph[1]], w=[hid])
                tb2 = tbank()
                for f in range(4):
                    P.op("pe", lambda E, f=f: E.transpose(tb2.t[:, f * 128:(f + 1) * 128], hid.t[:, f * 128:(f + 1) * 128], ident_bf.t[:]), r=[hid, ident_bf], w=[tb2])
                hidT = tmp("at_gdn", [128, 512], BF16)
                P.op("act", lambda E: E.copy(out=hidT.t[:], in_=tb2.t[:, 0:512]), r=[tb2], w=[hidT])
                ysb = tmp("z", [128, D], F32, 1) if e % 2 == 0 else tmp("xtok", [128, D], F32, 1)
                for half in range(2):
                    py = pbank()
                    for f in range(4):
                        P.op("pe", lambda E, f=f, py=py, half=half: E.matmul(py.t[:], lhsT=hidT.t[:, f * 128:(f + 1) * 128], rhs=dv[:, f, half * 512:(half + 1) * 512], start=(f == 0), stop=(f == 3)), r=[hidT, ws[2]], w=[py])
                    eng = "act" if half == 0 else "dve"
                    if eng == "act":
                        P.op("act", lambda E, py=py, half=half: E.copy(out=ysb.t[:, half * 512:(half + 1) * 512], in_=py.t[:]), r=[py], w=[ysb])
                    else:
                        P.op("dve", lambda E, py=py, half=half: E.tensor_copy(out=ysb.t[:, half * 512:(half + 1) * 512], in_=py.t[:]), r=[py], w=[ysb])
                P.dma("sp", lambda E: E.dma_start(out=ysc_d[e * CAP:(e + 1) * CAP, :], in_=ysb.t[:]), "yst", r=[ysb, ysc_T], n=2)

            ne = NEXP
            wq = {0: load_w(0)}
            if ne > 1:
                wq[1] = load_w(1)
            xgs = {0: gather(0)}
            for e in range(ne):
                if e + 1 < ne:
                    xgs[e + 1] = gather(e + 1)
                if e + 2 < ne:
                    wq[e + 2] = load_w(e + 2)
                expert(e, wq.pop(e), xgs.pop(e))

            P.dma("sp", lambda E: E.dma_start(out=lnp.t[:], in_=ln2_d), "ldd", w=[lnp])

            def layer_norm2(z, g_ap, b_ap, gb_tl):
                st = tmp("lnst", [128, 4], F32)
                junk = tmp("mixT", [128, 1024], BF16, 1)
                P.op("dve", lambda E: E.tensor_reduce(out=st.t[:, 0:1], in_=z.t[:], axis=AX.X, op=ALU.add), r=[z], w=[st])
                P.op("act", lambda E: E.activation(out=junk.t[:], in_=z.t[:], func=AF.Square, accum_out=st.t[:, 1:2]), r=[z], w=[junk, st])
                P.op("dve", lambda E: E.tensor_scalar(out=st.t[:, 0:2], in0=st.t[:, 0:2], scalar1=1.0 / D, scalar2=None, op0=ALU.mult), r=[st], w=[st])
                P.op("dve", lambda E: E.tensor_tensor(out=st.t[:, 2:3], in0=st.t[:, 0:1], in1=st.t[:, 0:1], op=ALU.mult), r=[st], w=[st])
                P.op("dve", lambda E: E.tensor_tensor(out=st.t[:, 2:3], in0=st.t[:, 1:2], in1=st.t[:, 2:3], op=ALU.subtract), r=[st], w=[st])
                P.op("act", lambda E: E.activation(out=st.t[:, 3:4], in_=st.t[:, 2:3], func=AF.Ln, bias=epsT.t[:, 1:2]), r=[st, epsT], w=[st])
                P.op("act", lambda E: E.activation(out=st.t[:, 3:4], in_=st.t[:, 3:4], func=AF.Exp, scale=-0.5), r=[st], w=[st])
                P.op("dve", lambda E: E.tensor_scalar(out=z.t[:], in0=z.t[:], scalar1=st.t[:, 0:1], scalar2=st.t[:, 3:4], op0=ALU.subtract, op1=ALU.mult), r=[z, st], w=[z])
                P.op("dve", lambda E: E.tensor_tensor(out=z.t[:], in0=z.t[:], in1=g_ap, op=ALU.mult), r=[z, gb_tl], w=[z])
                P.op("dve", lambda E: E.tensor_tensor(out=z.t[:], in0=z.t[:], in1=b_ap, op=ALU.add), r=[z, gb_tl], w=[z])

            class View:
                def __init__(self, ap):
                    self.t = ap
                    self.b = Buf()
            all_ws = [w_ for sl_ in wslots for w_ in sl_]
            f3 = [[View(big.t[:, (3 * i + j) * 2048:(3 * i + j + 1) * 2048].bitcast(F32)) for j in range(3)] for i in range(3)]
            f3_first = set()

            def final_load(tt):
                t0 = tt * 128
                acc, y0, y1 = f3[tt % 3]
                extra = []
                if tt % 3 not in f3_first and tt < 3:
                    extra = all_ws
                    f3_first.add(tt % 3)
                P.dma("sp", lambda E: E.dma_start(out=acc.t[:], in_=x1f_d[t0:t0 + 128, :]), "f0", r=([] if tt == 0 else [x1f_T]), w=([acc, x1f_T] if tt == 0 else [acc]) + extra, n=2)
                P.dma("pool", lambda E: E.indirect_dma_start(out=y0.t[:], out_offset=None, in_=ysc_d, in_offset=bass.IndirectOffsetOnAxis(ap=dest_i.t[:, 2 * tt:2 * tt + 1], axis=0)), "f1", r=([dest_i] if tt == 0 else [dest_i, ysc_T]), w=([y0, ysc_T] if tt == 0 else [y0]) + extra, n=2)
                P.dma("pool", lambda E: E.indirect_dma_start(out=y1.t[:], out_offset=None, in_=ysc_d, in_offset=bass.IndirectOffsetOnAxis(ap=dest_i.t[:, 2 * tt + 1:2 * tt + 2], axis=0)), "f2", r=[dest_i, ysc_T], w=[y1] + extra, n=2)

            def final_tile(tt):
                t0 = tt * 128
                acc, y0, y1 = f3[tt % 3]
                P.op("dve", lambda E: E.scalar_tensor_tensor(out=y0.t[:], in0=y0.t[:], scalar=gates.t[:, 2 * tt:2 * tt + 1], in1=y1.t[:], op0=ALU.mult, op1=ALU.bypass) if False else E.tensor_scalar(out=y0.t[:], in0=y0.t[:], scalar1=gates.t[:, 2 * tt:2 * tt + 1], scalar2=None, op0=ALU.mult), r=[y0, gates], w=[y0])
                P.op("dve", lambda E: E.scalar_tensor_tensor(out=y0.t[:], in0=y1.t[:], scalar=gates.t[:, 2 * tt + 1:2 * tt + 2], in1=y0.t[:], op0=ALU.mult, op1=ALU.add), r=[y1, gates, y0], w=[y0])
                P.op("dve", lambda E: E.scalar_tensor_tensor(out=acc.t[:], in0=acc.t[:], scalar=ALPHA, in1=y0.t[:], op0=ALU.mult, op1=ALU.add), r=[acc, y0], w=[acc])
                layer_norm2(acc, lnp.t[:, 0:D], lnp.t[:, D:2 * D], lnp)
                P.dma("sp", lambda E: E.dma_start(out=out_d[t0:t0 + 128, :], in_=acc.t[:]), "out", r=[acc], n=2)

            for tt in range(min(2, nt)):
                final_load(tt)
            for tt in range(nt):
                if tt + 2 < nt:
                    final_load(tt + 2)
                final_tile(tt)

        for key in list(P.dcount):
            P.wait_all_dma("sp", key.rstrip("0123456789"))
        P.emit()
    return nc


def prep_inputs(inputs):
    f = np.float32
    x = np.asarray(inputs["x"], f)
    small = np.zeros((128, PW), f)
    small[:, P_BGK:P_BGK + 2] = np.asarray(inputs["b_gk"][0], f).reshape(2, 128).T
    small[:, P_CONV:P_CONV + 48] = np.asarray(inputs["conv_w"][0], f).T.reshape(12, 128, 4).transpose(1, 0, 2).reshape(128, 48)
    small[:, P_ALOG:P_ALOG + 4] = np.asarray(inputs["a_log"][0], f)[None, :]
    small[:, P_DTB:P_DTB + 4] = np.asarray(inputs["dt_bias"][0], f)[None, :]
    small[:, P_RB:P_RB + 8] = np.asarray(inputs["b_router_group"][0], f)[None, :]
    small[:, P_RB + 8:P_RB + 72] = np.asarray(inputs["b_router_expert"][0], f)[None, :]
    gn = np.concatenate([np.tile(np.asarray(inputs["gla_norm_g"][0], f), 4), np.tile(np.asarray(inputs["gdn_norm_g"][0], f), 4)])
    gn = np.ascontiguousarray(np.broadcast_to(gn[None, :], (128, 1024)))
    ln1 = np.ascontiguousarray(np.broadcast_to(np.concatenate([inputs["ln1_g"][0], inputs["ln1_b"][0]]).astype(f)[None, :], (128, 2048)))
    ln2 = np.ascontiguousarray(np.broadcast_to(np.concatenate([inputs["ln2_g"][0], inputs["ln2_b"][0]]).astype(f)[None, :], (128, 2048)))
    w_r = np.ascontiguousarray(np.concatenate([inputs["w_router_group"][0], inputs["w_router_expert"][0]], axis=1).astype(f))
    shared = {
        "w_in": np.ascontiguousarray(inputs["w_in"][0], f),
        "w_out": np.ascontiguousarray(inputs["w_out"][0], f),
        "w_gk": np.ascontiguousarray(inputs["w_gk_up"][0], f),
        "small": small, "gn": gn, "ln1": ln1, "ln2": ln2, "consts": make_consts(), "w_r": w_r,
        "w_gate": np.ascontiguousarray(inputs["w_gate"][0], f),
        "w_up": np.ascontiguousarray(inputs["w_up"][0], f),
        "w_down": np.ascontiguousarray(inputs["w_down"][0], f),
    }
    maps = []
    for b in range(NCORES):
        m = dict(shared)
        m["x"] = np.ascontiguousarray(x[b])
        m["xT"] = np.ascontiguousarray(x[b].T)
        maps.append(m)
    return maps


def kernel(**inputs):
    maps = prep_inputs(inputs)
    nc = build()
    res = run_bass_kernel_spmd(nc, maps, core_ids=list(range(NCORES)))
    return np.stack([np.asarray(r["out"], np.float32) for r in res.results], axis=0)
```

```python
import numpy as np
from contextlib import ExitStack
import concourse.bass as bass
import concourse.mybir as mybir
from concourse.bass_utils import run_bass_kernel_spmd

F32 = mybir.dt.float32
BF16 = mybir.dt.bfloat16
I32 = mybir.dt.int32
AF = mybir.ActivationFunctionType
ALU = mybir.AluOpType
AX = mybir.AxisListType

D = 1024
S = 2048
NT = S // 128
NCORES = 8
INP = 3608
ALPHA = 2.0 ** 0.25
NEXP = 64
DEXP = 512
CAP = 128
YP = S + 1
NEG = -30000.0
SAME_ENGINE_SYNC = True
import os
CUT = int(os.environ.get("CUT", "0"))

O_QA, O_KA, O_VA, O_RA, O_LRA, O_QB, O_KB, O_VB, O_ZB, O_BB = 0, 256, 512, 1024, 1536, 1552, 2064, 2576, 3088, 3600

C_ID, C_ONE, C_MI, C_NM, C_ST, C_SL, C_U, C_IO, C_SC, C_TOK = 0, 128, 256, 384, 512, 640, 768, 896, 960, 1088
CW = 1089
P_BGK, P_CONV, P_ALOG, P_DTB, P_RB = 0, 2, 50, 54, 58
PW = 130


class Buf:
    __slots__ = ("w", "r")

    def __init__(self):
        self.w = None
        self.r = {}


class Tl:
    def __init__(self, t):
        self.t = t
        self.b = Buf()

    def __getitem__(self, k):
        return self.t[k]


class Op:
    __slots__ = ("eng", "fn", "waits", "signal", "value", "key", "ordinal", "isdma")


class Prog:
    ENGS = ("pe", "act", "dve", "pool", "sp")

    def __init__(self, nc, es):
        self.nc = nc
        self.es = es
        self.streams = {e: [] for e in self.ENGS}
        self.sems = {}
        self.dcount = {}
        self.waited = {}
        self.ecount = {e: 0 for e in self.ENGS}
        for e in self.ENGS:
            self.sems[e] = es.enter_context(nc.semaphore("s_" + e))
        self.nt = 0
        self.rot = {}
        self.finalized = set()
        self.last_dma = {}

    def dsem(self, name):
        if name not in self.sems:
            self.sems[name] = self.es.enter_context(self.nc.semaphore("d_" + name))
            self.dcount[name] = 0
        return name

    def sb(self, shape, dt, name=None):
        self.nt += 1
        return Tl(self.es.enter_context(self.nc.sbuf_tensor("sb_" + (name or f"t{self.nt}"), list(shape), dt)))

    def ps(self, shape, dt, name=None):
        self.nt += 1
        return Tl(self.es.enter_context(self.nc.psum_tensor("ps_" + (name or f"p{self.nt}"), list(shape), dt)))

    def _dep(self, op, prod):
        if prod is None:
            return
        if prod.key == op.eng and not prod.isdma:
            if op.eng == "pe" or not SAME_ENGINE_SYNC:
                return
        k = (op.eng, prod.key)
        if self.waited.get(k, 0) >= prod.ordinal:
            return
        self.waited[k] = prod.ordinal
        prod.signal = True
        op.waits.append(prod)

    def _mk(self, eng, fn, r, w, key, isdma):
        op = Op()
        op.eng, op.fn, op.waits, op.signal, op.value, op.key, op.isdma = eng, fn, [], isdma, 0, key, isdma
        if isdma:
            self.dcount[key] += 1
            op.ordinal = self.dcount[key]
        else:
            self.ecount[eng] += 1
            op.ordinal = self.ecount[eng]
        if isdma:
            self._dep(op, self.last_dma.get(key))
            self.last_dma[key] = op
        for t in r:
            self._dep(op, t.b.w)
        for t in w:
            self._dep(op, t.b.w)
            for p in list(t.b.r.values()):
                self._dep(op, p)
        for t in r:
            t.b.r[key] = op
        for t in w:
            t.b.w = op
            t.b.r = {}
        self.streams[eng].append(op)
        return op

    def op(self, eng, fn, r=(), w=()):
        return self._mk(eng, fn, r, w, eng, False)

    def dma(self, eng, fn, sem, r=(), w=(), n=1):
        st = self.rot.setdefault(sem, [0])
        key = self.dsem(f"{sem}{st[0] % n}")
        st[0] += 1
        return self._mk(eng, fn, r, w, key, True)

    def wait_all_dma(self, eng, sem):
        for key in list(self.dcount):
            if key.rstrip("0123456789") != sem or key in self.finalized:
                continue
            self.finalized.add(key)
            op = Op()
            op.eng, op.fn, op.waits, op.signal, op.value, op.key, op.isdma = eng, None, [], False, 0, eng, False
            op.ordinal = 0
            op.fn = ("wait", self.sems[key], 16 * self.dcount[key])
            self.streams[eng].append(op)

    def barrier(self):
        lasts = []
        for e in self.ENGS:
            for o in reversed(self.streams[e]):
                if isinstance(o.fn, tuple):
                    continue
                if not o.isdma:
                    lasts.append(o)
                    break
        dl = {}
        for e in self.ENGS:
            for o in self.streams[e]:
                if o.isdma:
                    dl[o.key] = o
        for e in self.ENGS:
            op = Op()
            op.eng, op.fn, op.waits, op.signal, op.value, op.key, op.isdma = e, ("nop",), [], False, 0, e, False
            op.ordinal = 0
            for p in lasts + list(dl.values()):
                if p.key == e and not p.isdma:
                    continue
                k = (e, p.key)
                if self.waited.get(k, 0) >= p.ordinal:
                    continue
                self.waited[k] = p.ordinal
                p.signal = True
                op.waits.append(p)
            self.streams[e].append(op)

    def emit(self):
        for e in self.ENGS:
            c = 0
            for o in self.streams[e]:
                if isinstance(o.fn, tuple):
                    continue
                if o.isdma:
                    o.value = 16 * o.ordinal
                elif o.signal:
                    c += 1
                    o.value = c
        engobj = {"pe": "tensor", "act": "scalar", "dve": "vector", "pool": "gpsimd", "sp": "sync"}
        with self.nc.Block() as block:
            for e in self.ENGS:
                stream = self.streams[e]
                sems = self.sems

                def body(E, stream=stream, e=e):
                    for o in stream:
                        for p in o.waits:
                            E.wait_ge(sems[p.key], p.value)
                        if isinstance(o.fn, tuple):
                            if o.fn[0] == "wait":
                                E.wait_ge(o.fn[1], o.fn[2])
                            continue
                        ins = o.fn(E)
                        if o.isdma:
                            ins.then_inc(sems[o.key], 16)
                        elif o.signal:
                            ins.then_inc(sems[e], 1)

                getattr(block, engobj[e])(body)


def make_consts():
    c = np.zeros((128, CW), np.float32)
    j = np.arange(128)[:, None]
    i = np.arange(128)[None, :]
    same = (j // 64) == (i // 64)
    c[:, C_ID:C_ID + 128] = (j == i)
    c[:, C_ONE:C_ONE + 128] = 1.0
    mi = same & (j <= i)
    c[:, C_MI:C_MI + 128] = mi
    c[:, C_NM:C_NM + 128] = np.where(mi, 0.0, NEG)
    c[:, C_ST:C_ST + 128] = same & (j < i)
    c[:, C_SL:C_SL + 128] = same & (j > i)
    c[:, C_U:C_U + 128] = (j < i)
    c[:, C_IO:C_IO + 64] = np.arange(64)[None, :]
    sc = np.ones(128, np.float32)
    sc[0] = 0.0
    sc[64] = 0.0
    c[:, C_SC:C_SC + 128] = sc[None, :]
    c[:, C_TOK] = np.arange(128)
    return c


def build(nt=NT, dbg=False, moe=True):
    nc = bass.Bass("TRN2", target_bir_lowering=False)

    def din(name, shape, dt=F32):
        return nc.dram_tensor(name, list(shape), dt, kind="ExternalInput").ap()

    xT_d = din("xT", [D, S])
    x_d = din("x", [S, D])
    win_d = din("w_in", [D, INP])
    wout_d = din("w_out", [D, D])
    wgk_d = din("w_gk", [16, 256])
    small_d = din("small", [128, PW])
    gn_d = din("gn", [128, 1024])
    ln1_d = din("ln1", [128, 2048])
    ln2_d = din("ln2", [128, 2048])
    consts_d = din("consts", [128, CW])
    wr_d = din("w_r", [D, 72])
    if moe:
        wg_d = din("w_gate", [NEXP, D, DEXP])
        wu_d = din("w_up", [NEXP, D, DEXP])
        wd_d = din("w_down", [NEXP, DEXP, D])
    out_d = nc.dram_tensor("out", [S, D], F32, kind="ExternalOutput").ap()
    if dbg:
        dbg_d = nc.dram_tensor("dbg", [S, D], F32, kind="ExternalOutput").ap()
    x1f_d = nc.dram_tensor("x1f", [S, D], F32, kind="Internal").ap()
    x1b_d = nc.dram_tensor("x1b", [S + 1, D], BF16, kind="Internal").ap()
    ysc_d = nc.dram_tensor("ysc", [NEXP * CAP + 128, D], F32, kind="Internal").ap()
    tab_d = nc.dram_tensor("tab", [NEXP * CAP + 128, 2], I32, kind="Internal").ap()

    with ExitStack() as es:
        P = Prog(nc, es)
        sb, ps = P.sb, P.ps

        big = sb([128, 8 * INP + 8 * D], BF16, "big")
        win_v = big.t[:, 0:8 * INP].rearrange("p (k c) -> p k c", k=8)
        wout_v = big.t[:, 8 * INP:8 * INP + 8 * D].rearrange("p (k c) -> p k c", k=8)
        cst = sb([128, CW], F32, "cst")
        small = sb([128, PW], F32, "small")
        gn = sb([128, 1024], F32, "gn")
        lnp = sb([128, 2048], F32, "lnp")
        wgk = sb([16, 256], F32, "wgk")
        wr = sb([128, 8, 72], F32, "wr")
        ident_bf = sb([128, 128], BF16, "ident_bf")
        ones_bf = sb([128, 128], BF16, "ones_bf")
        mask4_bf = sb([128, 512], BF16, "mask4")
        u_bf = sb([128, 128], BF16, "u_bf")
        negb = sb([128, 2], F32, "negb")
        negA = sb([128, 4], F32, "negA")
        epsT = sb([128, 2], F32, "epsT")
        hm = sb([128, 2], F32, "hm")
        P.op("pool", lambda E: E.memset(epsT.t[:, 0:1], 1e-6), w=[epsT])
        P.op("pool", lambda E: E.memset(epsT.t[:, 1:2], 1e-5), w=[epsT])

        ident = cst.t[:, C_ID:C_ID + 128]
        ones = cst.t[:, C_ONE:C_ONE + 128]
        m_incl = cst.t[:, C_MI:C_MI + 128]
        negmask = cst.t[:, C_NM:C_NM + 128]
        strict01 = cst.t[:, C_ST:C_ST + 128]
        strictL = cst.t[:, C_SL:C_SL + 128]
        scanmask = cst.t[:, C_SC:C_SC + 128]

        pbanks = [ps([128, 512], F32, f"pb{i}") for i in range(6)]
        tbanks = [ps([128, 1024], BF16, f"tb{i}") for i in range(2)]
        rr = {"p": 0, "t": 0}

        def pbank():
            rr["p"] = (rr["p"] + 1) % 5
            return pbanks[rr["p"]]

        def tbank():
            rr["t"] = (rr["t"] + 1) % 2
            return tbanks[rr["t"]]

        pools = {}

        def tmp(name, shape, dt, n=2):
            if name not in pools:
                pools[name] = [[sb(shape, dt, f"{name}_{i}") for i in range(n)], 0]
            pl = pools[name]
            pl[1] = (pl[1] + 1) % len(pl[0])
            return pl[0][pl[1]]

        def DD(name, tl, ncols, t0=0, rows=128):
            if dbg != name:
                return
            od = tmp("od", [128, 512], F32, 1)
            P.op("dve", lambda E: E.tensor_copy(out=od.t[0:rows, 0:ncols], in_=tl.t[0:rows, 0:ncols]), r=[tl], w=[od])
            P.dma("sp", lambda E: E.dma_start(out=dbg_d[t0:t0 + rows, 0:ncols], in_=od.t[0:rows, 0:ncols]), "dbg", r=[od])

        P.dma("sp", lambda E: E.dma_start(out=cst.t[:], in_=consts_d), "lda", w=[cst])
        P.dma("sp", lambda E: E.dma_start(out=small.t[:], in_=small_d), "ldb", w=[small])
        P.dma("sp", lambda E: E.dma_start(out=gn.t[:], in_=gn_d), "ldc", w=[gn])
        P.dma("sp", lambda E: E.dma_start(out=lnp.t[:], in_=ln1_d), "ldd", w=[lnp])
        P.dma("sp", lambda E: E.dma_start(out=wgk.t[:], in_=wgk_d), "lde", w=[wgk])
        P.dma("sp", lambda E: E.dma_start(out=wr.t[:], in_=wr_d.rearrange("(k p) c -> p k c", p=128)), "ldf", w=[wr])
        class DT0:
            def __init__(self):
                self.b = Buf()
        win_c = [DT0() for _ in range(8)]
        wout_c = [DT0() for _ in range(8)]
        for k in range(8):
            P.dma("pool", lambda E, k=k: E.dma_start(out=win_v[:, k, :], in_=win_d[k * 128:(k + 1) * 128, :]), "ldw", w=[win_c[k]], n=8)
        for k in range(8):
            P.dma("pool", lambda E, k=k: E.dma_start(out=wout_v[:, k, :], in_=wout_d[k * 128:(k + 1) * 128, :]), "ldw", w=[wout_c[k]], n=8)

        P.op("dve", lambda E: E.tensor_copy(out=ident_bf.t[:], in_=ident), r=[cst], w=[ident_bf])
        P.op("dve", lambda E: E.tensor_copy(out=ones_bf.t[:], in_=ones), r=[cst], w=[ones_bf])
        P.op("dve", lambda E: E.tensor_copy(out=u_bf.t[:], in_=cst.t[:, C_U:C_U + 128]), r=[cst], w=[u_bf])
        for h in range(4):
            P.op("dve", lambda E, h=h: E.tensor_copy(out=mask4_bf.t[:, h * 128:(h + 1) * 128], in_=m_incl), r=[cst], w=[mask4_bf])
        P.op("dve", lambda E: E.tensor_scalar(out=negb.t[:], in0=small.t[:, P_BGK:P_BGK + 2], scalar1=-1.0, scalar2=None, op0=ALU.mult), r=[small], w=[negb])
        P.op("act", lambda E: E.activation(out=negA.t[:], in_=small.t[:, P_ALOG:P_ALOG + 4], func=AF.Exp), r=[small], w=[negA])
        P.op("dve", lambda E: E.tensor_scalar(out=negA.t[:], in0=negA.t[:], scalar1=-1.0, scalar2=None, op0=ALU.mult), r=[negA], w=[negA])

        P.op("dve", lambda E: E.tensor_scalar(out=hm.t[:, 0:1], in0=cst.t[:, C_TOK:C_TOK + 1], scalar1=64.0, scalar2=None, op0=ALU.is_lt), r=[cst], w=[hm])
        P.op("dve", lambda E: E.tensor_scalar(out=hm.t[:, 1:2], in0=cst.t[:, C_TOK:C_TOK + 1], scalar1=64.0, scalar2=None, op0=ALU.is_ge), r=[cst], w=[hm])
        class DT:
            def __init__(self):
                self.b = Buf()
        x1f_T, x1b_T, tab_T, ysc_T = DT(), DT(), DT(), DT()
        gates = sb([128, 2 * NT], F32, "gates")
        dest_i = sb([128, 2 * NT], I32, "dest_i")
        Macc = sb([128, 64], BF16, "Macc")
        P.op("pool", lambda E: E.memset(Macc.t[:], 0.0), w=[Macc])
        if moe:
            tabinit = sb([128, 130], I32, "tabinit")
            P.op("pool", lambda E: E.memset(tabinit.t[:], S), w=[tabinit])
            P.dma("sp", lambda E: E.dma_start(out=tab_d.rearrange("(n p) c -> p n c", p=128), in_=tabinit.t[:].rearrange("p (n c) -> p n c", c=2)), "tabi", r=[tabinit], w=[tab_T])
        S_gla = [sb([128, 256], F32, f"S_gla{p}") for p in range(2)]
        S_gla_bf = [[sb([128, 256], BF16, f"S_gla_bf{p}_{i}") for i in range(3)] for p in range(2)]
        S_gdn = [sb([128, 128], F32, f"S_gdn{h}") for h in range(4)]
        S_gdn_bf = [[sb([128, 128], BF16, f"S_gdn_bf{h}_{i}") for i in range(3)] for h in range(4)]
        cin = [sb([128, 131], F32, f"cin{c}") for c in range(12)]
        qpad = [sb([128, 256], BF16, f"qpad{h}") for h in range(4)]
        vnews = [sb([128, 512], BF16, f"vnew{i}") for i in range(2)]
        kpad = [sb([128, 256], BF16, f"kpad{h}") for h in range(4)]
        qdpad = [sb([128, 256], BF16, f"qdpad{h}") for h in range(4)]
        for t in S_gla + S_gdn + cin:
            P.op("pool", lambda E, t=t: E.memset(t.t[:], 0.0), w=[t])
        for t in [x for l in S_gla_bf + S_gdn_bf for x in l] + qpad + kpad + qdpad + vnews:
            P.op("pool", lambda E, t=t: E.memset(t.t[:], 0.0), w=[t])

        if moe:
            for hlf in range(2):
                P.dma("sp", lambda E, hlf=hlf: E.dma_start(out=x1b_d[S:S + 1, hlf * 512:(hlf + 1) * 512], in_=vnews[0].t[0:1, :]), "zr", r=[vnews[0]], w=[x1b_T])

        def padview(t, rows=slice(0, 128)):
            return t.t[rows, :].rearrange("p (c x) -> p c x", c=2)[:, :, 64:128]

        def v2(ap):
            return ap.rearrange("p (c x) -> p c x", c=2)

        xT_tiles = {}

        def load_xT(tt):
            t0 = tt * 128
            xTt = tmp("xTt", [128, 8, 128], BF16, 2)
            P.dma("pool", lambda E: E.dma_start(out=xTt.t[:], in_=xT_d[:, t0:t0 + 128].rearrange("(k p) t -> p k t", p=128)), "ldx", w=[xTt], n=2)
            xT_tiles[tt] = xTt

        def gdn_proj_steps(xTs):
            steps = []
            for kind, col in (("q", O_QB), ("k", O_KB), ("v", O_VB)):
                for h in range(4):
                    cb = cin[{"q": 0, "k": 4, "v": 8}[kind] + h]
                    c0 = col + h * 128

                    def step(cb=cb, c0=c0):
                        pb = pbank()
                        for k in range(8):
                            P.op("pe", lambda E, k=k: E.matmul(pb.t[:, 0:128], lhsT=win_v[:, k, c0:c0 + 128], rhs=xTs.t[:, k, :], start=(k == 0), stop=(k == 7)), r=[win_c[k], xTs], w=[pb])
                        P.op("act", lambda E: E.copy(out=cb.t[:, 3:131], in_=pb.t[:, 0:128]), r=[pb], w=[cb])
                    steps.append(step)
            return steps

        def tile_body(tt, prev_route=None):
            t0 = tt * 128
            xTt = xT_tiles.pop(tt)
            if tt + 1 < nt:
                load_xT(tt + 1)
            xtok = tmp("xtok", [128, D], F32, 1)

            def fm_proj(pb, col0, m, off):
                for k in range(8):
                    P.op("pe", lambda E, k=k: E.matmul(pb.t[0:m, off:off + 128], lhsT=win_v[:, k, col0:col0 + m], rhs=xTt.t[:, k, :], start=(k == 0), stop=(k == 7)), r=[win_c[k], xTt], w=[pb])

            def tm_proj(pb, col0, n):
                for k in range(8):
                    P.op("pe", lambda E, k=k: E.matmul(pb.t[:, 0:n], lhsT=xTt.t[:, k, :], rhs=win_v[:, k, col0:col0 + n], start=(k == 0), stop=(k == 7)), r=[win_c[k], xTt], w=[pb])

            if tt == 0:
                for st_ in gdn_proj_steps(xTt):
                    st_()
            hoist = gdn_proj_steps(xT_tiles[tt + 1]) if tt + 1 < nt else []

            pb_s = pbank()
            tm_proj(pb_s, O_BB, 8)
            sc1 = tmp("sc1", [128, 8], F32)
            P.op("act", lambda E: E.activation(out=sc1.t[:, 0:4], in_=pb_s.t[:, 0:4], func=AF.Exp, scale=-1.0), r=[pb_s], w=[sc1])
            P.op("dve", lambda E: E.tensor_tensor(out=sc1.t[:, 4:8], in0=pb_s.t[:, 4:8], in1=small.t[:, P_DTB:P_DTB + 4], op=ALU.add), r=[pb_s, small], w=[sc1])
            beta = tmp("beta", [128, 4], F32)
            nbeta = tmp("nbeta", [128, 4], F32)
            P.op("dve", lambda E: E.tensor_scalar(out=sc1.t[:, 0:4], in0=sc1.t[:, 0:4], scalar1=1.0, scalar2=None, op0=ALU.add), r=[sc1], w=[sc1])
            P.op("dve", lambda E: E.reciprocal(out=beta.t[:], in_=sc1.t[:, 0:4]), r=[sc1], w=[beta])
            P.op("dve", lambda E: E.tensor_scalar(out=nbeta.t[:], in0=beta.t[:], scalar1=-1.0, scalar2=None, op0=ALU.mult), r=[beta], w=[nbeta])
            sc2 = tmp("sc2", [128, 4], F32)
            P.op("act", lambda E: E.activation(out=sc2.t[:], in_=sc1.t[:, 4:8], func=AF.Exp), r=[sc1], w=[sc2])
            P.op("act", lambda E: E.activation(out=sc2.t[:], in_=sc2.t[:], func=AF.Ln, bias=1.0), r=[sc2], w=[sc2])
            gtk = tmp("gtk", [128, 4], F32)
            P.op("dve", lambda E: E.tensor_tensor(out=gtk.t[:], in0=sc2.t[:], in1=negA.t[:], op=ALU.mult), r=[sc2, negA], w=[gtk])
            va_tok = tmp("va_tok", [128, 512], BF16)
            pb = pbank()
            tm_proj(pb, O_VA, 512)
            P.op("act", lambda E, pb=pb: E.copy(out=va_tok.t[:], in_=pb.t[:]), r=[pb], w=[va_tok])
            DD("va", va_tok, 512, t0)
            gate = tmp("gate", [128, 1024], F32, 1)
            for gi, col in enumerate((O_RA, O_ZB)):
                pb = pbank()
                tm_proj(pb, col, 512)
                P.op("act", lambda E, pb=pb, gi=gi: E.activation(out=gate.t[:, gi * 512:(gi + 1) * 512], in_=pb.t[:], func=AF.Silu), r=[pb], w=[gate])
            P.op("pool", lambda E: E.tensor_tensor(out=gate.t[:], in0=gate.t[:], in1=gn.t[:], op=ALU.mult), r=[gate, gn], w=[gate])

            pb_l = pbank()
            fm_proj(pb_l, O_LRA, 16, 0)
            lraT = tmp("lraT", [16, 128], F32)
            P.op("act", lambda E: E.copy(out=lraT.t[:], in_=pb_l.t[0:16, 0:128]), r=[pb_l], w=[lraT])
            pb_qk = pbanks[5]
            for i, col in enumerate((O_QA, O_QA + 128, O_KA, O_KA + 128)):
                fm_proj(pb_qk, col, 128, i * 128)
            if prev_route is not None:
                prev_route[2]()
            P.dma("sp", lambda E, xtok=xtok, t0=t0: E.dma_start(out=xtok.t[:], in_=x_d[t0:t0 + 128, :]), "ldx2", w=[xtok])
            qn = tmp("qn", [128, 512], BF16)
            vT = tmp("vT", [128, 512], BF16)
            kn = tmp("kn", [128, 512], BF16)
            rnb = tmp("rn", [128, 512], F32, 1)
            cvs = {"q": tmp("cv", [128, 512], F32, 2), "k": tmp("cv", [128, 512], F32, 2), "v": rnb}
            for kind in ("q", "k", "v"):
                cv = cvs[kind]
                for h in range(4):
                    ci = {"q": 0, "k": 4, "v": 8}[kind] + h
                    cb = cin[ci]
                    o = cv.t[:, h * 128:(h + 1) * 128]
                    wc = P_CONV + ci * 4
                    P.op("dve", lambda E, o=o, cb=cb, wc=wc: E.tensor_scalar(out=o, in0=cb.t[:, 0:128], scalar1=small.t[:, wc:wc + 1], scalar2=None, op0=ALU.mult), r=[cb, small], w=[cv])
                    for i in (1, 2, 3):
                        P.op("dve", lambda E, o=o, cb=cb, wc=wc, i=i: E.scalar_tensor_tensor(out=o, in0=cb.t[:, i:i + 128], scalar=small.t[:, wc + i:wc + i + 1], in1=o, op0=ALU.mult, op1=ALU.add), r=[cb, small, cv], w=[cv])
                    P.op("pool", lambda E, cb=cb: E.tensor_copy(out=cb.t[:, 0:3], in_=cb.t[:, 128:131]), r=[cb], w=[cb])
            P.op("act", lambda E: E.activation(out=cvs["q"].t[:], in_=cvs["q"].t[:], func=AF.Silu), r=[cvs["q"]], w=[cvs["q"]])
            P.op("act", lambda E: E.activation(out=cvs["k"].t[:], in_=cvs["k"].t[:], func=AF.Silu), r=[cvs["k"]], w=[cvs["k"]])
            P.op("act", lambda E: E.activation(out=vT.t[:], in_=cvs["v"].t[:], func=AF.Silu), r=[cvs["v"]], w=[vT])
            pssd = {}
            for kind in ("q", "k"):
                cv = cvs[kind]
                sq = tmp("sq", [128, 512], BF16, 1)
                P.op("pool", lambda E, cv=cv, sq=sq: E.tensor_tensor(out=sq.t[:], in0=cv.t[:], in1=cv.t[:], op=ALU.mult), r=[cv], w=[sq])
                pss = pbank()
                P.op("pe", lambda E, pss=pss, sq=sq: E.matmul(pss.t[:], lhsT=ones_bf.t[:], rhs=sq.t[:], start=True, stop=True), r=[ones_bf, sq], w=[pss])
                pssd[kind] = pss
            for kind in ("q", "k"):
                cv = cvs[kind]
                pss = pssd[kind]
                rn = rnb
                P.op("act", lambda E, pss=pss, rn=rn: E.activation(out=rn.t[:], in_=pss.t[:], func=AF.Ln, bias=epsT.t[:, 0:1]), r=[pss, epsT], w=[rn])
                P.op("act", lambda E, rn=rn: E.activation(out=rn.t[:], in_=rn.t[:], func=AF.Exp, scale=-0.5), r=[rn], w=[rn])
                if kind == "q":
                    P.op("dve", lambda E, cv=cv, rn=rn: E.scalar_tensor_tensor(out=qn.t[:], in0=cv.t[:], scalar=128.0 ** -0.5, in1=rn.t[:], op0=ALU.mult, op1=ALU.mult), r=[cv, rn], w=[qn])
                else:
                    P.op("dve", lambda E, cv=cv, rn=rn: E.tensor_tensor(out=kn.t[:], in0=cv.t[:], in1=rn.t[:], op=ALU.mult), r=[cv, rn], w=[kn])
                    for h in range(4):
                        P.op("pool", lambda E, h=h: E.tensor_copy(out=padview(kpad[h]), in_=v2(kn.t[:, h * 128:(h + 1) * 128])), r=[kn], w=[kpad[h]])
            k_tok = tmp("k_tok", [128, 512], BF16)
            v_tok = tmp("v_tok", [128, 512], BF16)
            kdec = tmp("kdec", [128, 512], BF16)
            tb = tbank()
            for h in range(4):
                P.op("pe", lambda E, tb=tb, h=h: E.transpose(tb.t[:, h * 128:(h + 1) * 128], kn.t[:, h * 128:(h + 1) * 128], ident_bf.t[:]), r=[kn, ident_bf], w=[tb])
            P.op("act", lambda E, tb=tb: E.copy(out=k_tok.t[:], in_=tb.t[:, 0:512]), r=[tb], w=[k_tok])
            tb2 = tbank()
            for h in range(4):
                P.op("pe", lambda E, tb2=tb2, h=h: E.transpose(tb2.t[:, h * 128:(h + 1) * 128], vT.t[:, h * 128:(h + 1) * 128], ident_bf.t[:]), r=[vT, ident_bf], w=[tb2])
            P.op("act", lambda E, tb2=tb2: E.copy(out=v_tok.t[:], in_=tb2.t[:, 0:512]), r=[tb2], w=[v_tok])

            pb_g = pbank()
            P.op("pe", lambda E: E.matmul(pb_g.t[:, 0:4], lhsT=m_incl, rhs=gtk.t[:], start=True, stop=True), r=[cst, gtk], w=[pb_g])
            P.op("pe", lambda E: E.matmul(pb_g.t[:, 4:8], lhsT=strictL, rhs=gtk.t[:], start=True, stop=True), r=[cst, gtk], w=[pb_g])
            gg = tmp("gg", [128, 8], F32)
            P.op("dve", lambda E: E.tensor_copy(out=gg.t[:], in_=pb_g.t[:, 0:8]), r=[pb_g], w=[gg])
            eg = tmp("eg", [128, 8], F32)
            P.op("act", lambda E: E.activation(out=eg.t[:], in_=gg.t[:], func=AF.Exp), r=[gg], w=[eg])
            neg_eg = tmp("neg_eg", [128, 4], F32)
            P.op("dve", lambda E: E.tensor_scalar(out=neg_eg.t[:], in0=eg.t[:, 0:4], scalar1=-1.0, scalar2=None, op0=ALU.mult), r=[eg], w=[neg_eg])
            D4 = tmp("D4", [128, 512], F32, 1)
            for h in range(4):
                P.op("act", lambda E, h=h: E.activation(out=D4.t[:, h * 128:(h + 1) * 128], in_=ident, func=AF.Copy, scale=gg.t[:, h:h + 1]), r=[cst, gg], w=[D4])
            pb_G = pbank()
            P.op("pe", lambda E: E.matmul(pb_G.t[:], lhsT=ones, rhs=D4.t[:], start=True, stop=True), r=[cst, D4], w=[pb_G])
            Grow = tmp("Grow", [128, 512], F32, 1)
            P.op("act", lambda E: E.copy(out=Grow.t[:], in_=pb_G.t[:]), r=[pb_G], w=[Grow])
            eG = tmp("eG", [128, 512], F32, 1)
            P.op("act", lambda E: E.activation(out=eG.t[:], in_=pb_G.t[:], func=AF.Exp), r=[pb_G], w=[eG])

            if CUT == 1:
                return
            mixed = tmp("mixed", [128, 1024], BF16, 1)

            def norm_gate(gi, po):
                sq2 = tmp("sq2", [128, 512], F32, 1)
                P.op("act", lambda E: E.activation(out=sq2.t[:], in_=po.t[:], func=AF.Square), r=[po], w=[sq2])
                ssq = tmp("ssq", [128, 4], F32)
                P.op("dve", lambda E: E.tensor_reduce(out=ssq.t[:], in_=sq2.t[:].rearrange("p (h e) -> p h e", h=4), axis=AX.X, op=ALU.add), r=[sq2], w=[ssq])
                P.op("act", lambda E: E.activation(out=ssq.t[:], in_=ssq.t[:], func=AF.Ln, scale=1.0 / 128.0, bias=epsT.t[:, 0:1]), r=[ssq, epsT], w=[ssq])
                P.op("act", lambda E: E.activation(out=ssq.t[:], in_=ssq.t[:], func=AF.Exp, scale=-0.5), r=[ssq], w=[ssq])
                for h in range(4):
                    hs = slice(h * 128, (h + 1) * 128)
                    gs = slice(gi * 512 + h * 128, gi * 512 + (h + 1) * 128)
                    P.op("dve", lambda E, hs=hs, gs=gs, h=h: E.scalar_tensor_tensor(out=mixed.t[:, gs], in0=po.t[:, hs], scalar=ssq.t[:, h:h + 1], in1=gate.t[:, gs], op0=ALU.mult, op1=ALU.mult), r=[po, ssq, gate], w=[mixed])

            if CUT == 2:
                return
            pb_gk = pbank()
            for p in range(2):
                P.op("pe", lambda E, p=p: E.matmul(pb_gk.t[:, p * 128:(p + 1) * 128], lhsT=wgk.t[:, p * 128:(p + 1) * 128], rhs=lraT.t[:], start=True, stop=True), r=[wgk, lraT], w=[pb_gk])
            khat_tok = tmp("khat_tok", [128, 256], BF16)
            ktil = []
            ktms = []
            qtil = []
            eb = []
            for p in range(2):
                l1 = tmp("l1", [128, 128], F32)
                P.op("act", lambda E, p=p, l1=l1: E.activation(out=l1.t[:], in_=pb_gk.t[:, p * 128:(p + 1) * 128], func=AF.Exp, scale=-1.0, bias=negb.t[:, p:p + 1]), r=[pb_gk, negb], w=[l1])
                P.op("act", lambda E, l1=l1: E.activation(out=l1.t[:], in_=l1.t[:], func=AF.Ln, bias=1.0), r=[l1], w=[l1])
                cs = tmp("cs", [128, 128], F32)
                P.op("dve", lambda E, l1=l1, cs=cs: E.tensor_tensor_scan(out=cs.t[:], data0=scanmask, data1=l1.t[:], initial=0.0, op0=ALU.mult, op1=ALU.add), r=[l1, cst], w=[cs])
                ebT = tmp("ebT", [128, 128], F32, 2)
                P.op("act", lambda E, cs=cs, ebT=ebT: E.activation(out=ebT.t[:], in_=cs.t[:], func=AF.Exp, scale=-1.0 / 16.0), r=[cs], w=[ebT])
                enbT = tmp("enbT", [128, 128], F32)
                P.op("act", lambda E, cs=cs, enbT=enbT: E.activation(out=enbT.t[:], in_=cs.t[:], func=AF.Exp, scale=1.0 / 16.0), r=[cs], w=[enbT])
                sfx = tmp("sfx", [128, 128], F32)
                for c in range(2):
                    P.op("dve", lambda E, cs=cs, sfx=sfx, c=c: E.tensor_scalar(out=sfx.t[:, c * 64:(c + 1) * 64], in0=cs.t[:, c * 64:(c + 1) * 64], scalar1=cs.t[:, c * 64 + 63:c * 64 + 64], scalar2=None, op0=ALU.subtract), r=[cs], w=[sfx])
                P.op("act", lambda E, sfx=sfx: E.activation(out=sfx.t[:], in_=sfx.t[:], func=AF.Exp, scale=1.0 / 16.0), r=[sfx], w=[sfx])
                qt = tmp("qtil", [128, 128], BF16, 2)
                P.op("dve", lambda E, p=p, ebT=ebT, qt=qt: E.scalar_tensor_tensor(out=qt.t[:], in0=pb_qk.t[:, p * 128:(p + 1) * 128], scalar=0.125, in1=ebT.t[:], op0=ALU.mult, op1=ALU.mult), r=[pb_qk, ebT], w=[qt])
                for hp in range(2):
                    P.op("dve", lambda E, p=p, qt=qt, hp=hp: E.tensor_scalar(out=padview(qpad[2 * p + hp]), in0=v2(qt.t[:]), scalar1=hm.t[:, hp:hp + 1], scalar2=None, op0=ALU.mult), r=[qt, hm], w=[qpad[2 * p + hp]])
                qtil.append(qt)
                kt = tmp("ktil", [128, 128], BF16, 2)
                P.op("dve", lambda E, p=p, kt=kt, enbT=enbT: E.tensor_tensor(out=kt.t[:], in0=pb_qk.t[:, 256 + p * 128:256 + (p + 1) * 128], in1=enbT.t[:], op=ALU.mult), r=[pb_qk, enbT], w=[kt])
                for hp in range(2):
                    ktm = tmp("ktm", [128, 128], BF16, 4)
                    P.op("dve", lambda E, kt=kt, hp=hp, ktm=ktm: E.tensor_scalar(out=ktm.t[:], in0=kt.t[:], scalar1=hm.t[:, hp:hp + 1], scalar2=None, op0=ALU.mult), r=[kt, hm], w=[ktm])
                    ktms.append(ktm)
                khT = tmp("khT", [128, 128], BF16)
                P.op("dve", lambda E, p=p, khT=khT, sfx=sfx: E.tensor_tensor(out=khT.t[:], in0=pb_qk.t[:, 256 + p * 128:256 + (p + 1) * 128], in1=sfx.t[:], op=ALU.mult), r=[pb_qk, sfx], w=[khT])
                tb = tbank()
                P.op("pe", lambda E, tb=tb, khT=khT: E.transpose(tb.t[:, 0:128], khT.t[:], ident_bf.t[:]), r=[khT, ident_bf], w=[tb])
                P.op("act", lambda E, tb=tb, p=p: E.copy(out=khat_tok.t[:, p * 128:(p + 1) * 128], in_=tb.t[:, 0:128]), r=[tb], w=[khat_tok])
                ktil.append(kt)
                eb.append(ebT)

            DD("qtil0", qtil[0], 128, t0); DD("ktil0", ktil[0], 128, t0); DD("eb0", eb[0], 128, t0); DD("khat", khat_tok, 256, t0)
            if CUT == 3:
                return
            pb_at = pbank()
            for h in range(4):
                p, hp = h // 2, h % 2
                rows = slice(hp * 64, hp * 64 + 64)
                P.op("pe", lambda E, h=h, p=p, rows=rows: E.matmul(pb_at.t[:, h * 128:(h + 1) * 128], lhsT=ktms[h].t[:, :], rhs=qtil[p].t[:, :], start=True, stop=True), r=[ktms[h], qtil[p]], w=[pb_at])
            at_gla = tmp("at_gla", [128, 512], BF16)
            P.op("dve", lambda E: E.tensor_tensor(out=at_gla.t[:], in0=pb_at.t[:], in1=mask4_bf.t[:], op=ALU.mult), r=[pb_at, mask4_bf], w=[at_gla])
            DD("atgla", at_gla, 512, t0)
            if CUT == 31:
                return
            si = (2 * tt) % 3
            khat_m = []
            for c in range(2):
                km = tmp("khat_m", [128, 256], BF16, 2)
                P.op("dve", lambda E, km=km, c=c: E.tensor_scalar(out=km.t[:], in0=khat_tok.t[:], scalar1=hm.t[:, c:c + 1], scalar2=None, op0=ALU.mult), r=[khat_tok, hm], w=[km])
                khat_m.append(km)
            for p in range(2):
                for c in range(2):
                    pu = pbank()
                    crow = slice(c * 64, c * 64 + 64)
                    P.op("pe", lambda E, p=p, pu=pu, c=c: E.matmul(pu.t[:, 0:256], lhsT=khat_m[c].t[:, p * 128:(p + 1) * 128], rhs=va_tok.t[:, p * 256:(p + 1) * 256], start=True, stop=True), r=[khat_m[c], va_tok], w=[pu])
                    P.op("dve", lambda E, p=p, pu=pu, c=c: E.scalar_tensor_tensor(out=S_gla[p].t[:], in0=S_gla[p].t[:], scalar=eb[p].t[:, c * 64 + 63:c * 64 + 64], in1=pu.t[:, 0:256], op0=ALU.mult, op1=ALU.add), r=[S_gla[p], eb[p], pu], w=[S_gla[p]])
                    sdst = S_gla_bf[p][(si + c + 1) % 3]
                    P.op("act", lambda E, p=p, sdst=sdst: E.copy(out=sdst.t[:], in_=S_gla[p].t[:]), r=[S_gla[p]], w=[sdst])
            if CUT == 32:
                return
            if CUT == 4:
                return
            for h in range(4):
                P.op("act", lambda E, h=h: E.activation(out=kdec.t[:, h * 128:(h + 1) * 128], in_=k_tok.t[:, h * 128:(h + 1) * 128], func=AF.Copy, scale=eg.t[:, 4 + h:5 + h]), r=[k_tok, eg], w=[kdec])

            DD("kn", kn, 512, t0); DD("qn", qn, 512, t0); DD("vtok", v_tok, 512, t0); DD("ktok", k_tok, 512, t0); DD("gg", gg, 8, t0); DD("eG", eG, 512, t0)
            if CUT == 5:
                return
            pb_kk = pbank()
            pb_qk2 = pbank()
            for h in range(4):
                P.op("pe", lambda E, h=h: E.matmul(pb_kk.t[:, h * 128:(h + 1) * 128], lhsT=kn.t[:, h * 128:(h + 1) * 128], rhs=kn.t[:, h * 128:(h + 1) * 128], start=True, stop=True), r=[kn], w=[pb_kk])
                P.op("pe", lambda E, h=h: E.matmul(pb_qk2.t[:, h * 128:(h + 1) * 128], lhsT=kn.t[:, h * 128:(h + 1) * 128], rhs=qn.t[:, h * 128:(h + 1) * 128], start=True, stop=True), r=[kn, qn], w=[pb_qk2])
            dec = tmp("dec", [128, 512], F32, 1)
            for h in range(4):
                hs = slice(h * 128, (h + 1) * 128)
                P.op("dve", lambda E, h=h, hs=hs: E.scalar_tensor_tensor(out=dec.t[:, hs], in0=Grow.t[:, hs], scalar=gg.t[:, h:h + 1], in1=negmask, op0=ALU.subtract, op1=ALU.add), r=[Grow, gg, cst], w=[dec])
            P.op("act", lambda E: E.activation(out=dec.t[:], in_=dec.t[:], func=AF.Exp), r=[dec], w=[dec])
            at_gdn = tmp("at_gdn", [128, 512], BF16)
            P.op("dve", lambda E: E.tensor_tensor(out=at_gdn.t[:], in0=pb_qk2.t[:], in1=dec.t[:], op=ALU.mult), r=[pb_qk2, dec], w=[at_gdn])
            Y = tmp("Y", [128, 512], F32, 2)
            X = tmp("X", [128, 512], F32, 2)
            Pm = tmp("Pm", [128, 512], F32, 2)
            for h in range(4):
                hs = slice(h * 128, (h + 1) * 128)
                P.op("pool", lambda E, hs=hs: E.tensor_tensor(out=dec.t[:, hs], in0=dec.t[:, hs], in1=strict01, op=ALU.mult), r=[dec, cst], w=[dec])
                P.op("dve", lambda E, h=h, hs=hs, Y=Y: E.scalar_tensor_tensor(out=Y.t[:, hs], in0=pb_kk.t[:, hs], scalar=nbeta.t[:, h:h + 1], in1=dec.t[:, hs], op0=ALU.mult, op1=ALU.mult), r=[pb_kk, nbeta, dec], w=[Y])
            for h in range(4):
                hs = slice(h * 128, (h + 1) * 128)
                P.op("dve", lambda E, h=h, hs=hs: E.tensor_tensor(out=padview(qdpad[h]), in0=v2(qn.t[:, hs]), in1=v2(eG.t[:, hs]), op=ALU.mult), r=[qn, eG], w=[qdpad[h]])
            pbx = pbank()
            for h in range(4):
                hs = slice(h * 128, (h + 1) * 128)
                P.op("pe", lambda E, hs=hs, Y=Y: E.transpose(pbx.t[:, hs], Y.t[:, hs], ident), r=[Y, cst], w=[pbx])
            P.op("act", lambda E, X=X: E.copy(out=X.t[:], in_=pbx.t[:]), r=[pbx], w=[X])
            for h in range(4):
                P.op("pool", lambda E, Pm=Pm, Y=Y, h=h: E.tensor_tensor(out=Pm.t[:, h * 128:(h + 1) * 128], in0=Y.t[:, h * 128:(h + 1) * 128], in1=ident, op=ALU.add), r=[Y, cst], w=[Pm])
            po_gla = pbank()
            for h in range(4):
                p, hp = h // 2, h % 2
                rows = slice(hp * 64, hp * 64 + 64)
                for c in range(2):
                    ssrc = S_gla_bf[p][(si + c) % 3]
                    P.op("pe", lambda E, h=h, p=p, c=c, hp=hp, rows=rows, ssrc=ssrc: E.matmul(po_gla.t[:, h * 128:(h + 1) * 128], lhsT=qpad[h].t[:, 64 + c * 64:192 + c * 64], rhs=ssrc.t[:, hp * 128:(hp + 1) * 128], start=(c == 0), stop=False), r=[qpad[h], ssrc], w=[po_gla])
                P.op("pe", lambda E, h=h: E.matmul(po_gla.t[:, h * 128:(h + 1) * 128], lhsT=at_gla.t[:, h * 128:(h + 1) * 128], rhs=va_tok.t[:, h * 128:(h + 1) * 128], start=False, stop=True), r=[at_gla, va_tok], w=[po_gla])

            if CUT == 33:
                return
            DD("ogla", po_gla, 512, t0)
            norm_gate(0, po_gla)

            TT = tmp("TT", [128, 512], BF16)
            for lvl in range(1, 6):
                pX = pbank()
                for h in range(4):
                    hs = slice(h * 128, (h + 1) * 128)
                    P.op("pe", lambda E, hs=hs, pX=pX, X=X, Y=Y: E.matmul(pX.t[:, hs], lhsT=Y.t[:, hs], rhs=X.t[:, hs], start=True, stop=True), r=[X, Y], w=[pX])
                if hoist:
                    hoist.pop(0)()
                Xn = tmp("X", [128, 512], F32, 2)
                if lvl < 5:
                    pY = pbank()
                    for h in range(4):
                        hs = slice(h * 128, (h + 1) * 128)
                        P.op("pe", lambda E, hs=hs, pY=pY, X=X, Y=Y: E.matmul(pY.t[:, hs], lhsT=X.t[:, hs], rhs=Y.t[:, hs], start=True, stop=True), r=[X, Y], w=[pY])
                    if hoist:
                        hoist.pop(0)()
                    Yn = tmp("Y", [128, 512], F32, 2)
                    P.op("act", lambda E, pY=pY, Yn=Yn: E.copy(out=Yn.t[:], in_=pY.t[:]), r=[pY], w=[Yn])
                    Y = Yn
                P.op("dve", lambda E, pX=pX, Xn=Xn: E.tensor_copy(out=Xn.t[:], in_=pX.t[:]), r=[pX], w=[Xn])
                X = Xn
                pP = pbank()
                for h in range(4):
                    hs = slice(h * 128, (h + 1) * 128)
                    P.op("pe", lambda E, hs=hs, pP=pP, X=X, Pm=Pm: E.matmul(pP.t[:, hs], lhsT=X.t[:, hs], rhs=Pm.t[:, hs], start=True, stop=True), r=[X, Pm], w=[pP])
                if hoist:
                    hoist.pop(0)()
                if lvl < 5:
                    Pn = tmp("Pm", [128, 512], F32, 2)
                    P.op("dve", lambda E, pP=pP, Pn=Pn, Pm=Pm: E.tensor_tensor(out=Pn.t[:], in0=pP.t[:], in1=Pm.t[:], op=ALU.add), r=[pP, Pm], w=[Pn])
                    Pm = Pn
                else:
                    P.op("dve", lambda E, pP=pP, Pm=Pm: E.tensor_tensor(out=TT.t[:], in0=pP.t[:], in1=Pm.t[:], op=ALU.add), r=[pP, Pm], w=[TT])

            DD("dec", dec, 512, t0); DD("TT", TT, 512, t0); DD("atgdn", at_gdn, 512, t0)
            if CUT == 6:
                return
            while hoist:
                hoist.pop(0)()
            vnew = vnews[tt % 2]
            kdec_m = []
            for c in range(2):
                km = tmp("kdec_m", [128, 512], BF16, 2)
                P.op("dve", lambda E, km=km, c=c: E.tensor_scalar(out=km.t[:], in0=kdec.t[:], scalar1=hm.t[:, c:c + 1], scalar2=None, op0=ALU.mult), r=[kdec, hm], w=[km])
                kdec_m.append(km)
            for c in range(2):
                crow = slice(c * 64, c * 64 + 64)
                pk = pbank()
                for h in range(4):
                    hs = slice(h * 128, (h + 1) * 128)
                    ssrc = S_gdn_bf[h][(si + c) % 3]
                    P.op("pe", lambda E, h=h, hs=hs, c=c, pk=pk, ssrc=ssrc: E.matmul(pk.t[:, hs], lhsT=kpad[h].t[:, 64 + c * 64:192 + c * 64], rhs=ssrc.t[:], start=True, stop=True), r=[kpad[h], ssrc], w=[pk])
                rr_ = tmp("rr", [128, 512], BF16)
                for h in range(4):
                    hs = slice(h * 128, (h + 1) * 128)
                    P.op("dve", lambda E, h=h, hs=hs, pk=pk, rr_=rr_: E.scalar_tensor_tensor(out=rr_.t[:, hs], in0=pk.t[:, hs], scalar=neg_eg.t[:, h:h + 1], in1=v_tok.t[:, hs], op0=ALU.mult, op1=ALU.add), r=[pk, neg_eg, v_tok], w=[rr_])
                pv = pbank()
                for h in range(4):
                    hs = slice(h * 128, (h + 1) * 128)
                    P.op("pe", lambda E, hs=hs, pv=pv, rr_=rr_, crow=crow: E.matmul(pv.t[:, hs], lhsT=TT.t[:, hs], rhs=rr_.t[:, hs], start=True, stop=True), r=[TT, rr_], w=[pv])
                for h in range(4):
                    hs = slice(h * 128, (h + 1) * 128)
                    P.op("act", lambda E, h=h, hs=hs, pv=pv, crow=crow: E.activation(out=vnew.t[crow, hs], in_=pv.t[crow, hs], func=AF.Copy, scale=beta.t[crow, h:h + 1]), r=[pv, beta], w=[vnew])
                for h in range(4):
                    hs = slice(h * 128, (h + 1) * 128)
                    pu = pbank()
                    P.op("pe", lambda E, hs=hs, pu=pu, c=c: E.matmul(pu.t[:, 0:128], lhsT=kdec_m[c].t[:, hs], rhs=vnew.t[:, hs], start=True, stop=True), r=[kdec_m[c], vnew], w=[pu])
                    dcol = h * 128 + c * 64 + 63
                    P.op("dve", lambda E, h=h, pu=pu, dcol=dcol: E.scalar_tensor_tensor(out=S_gdn[h].t[:], in0=S_gdn[h].t[:], scalar=eG.t[:, dcol:dcol + 1], in1=pu.t[:, 0:128], op0=ALU.mult, op1=ALU.add), r=[S_gdn[h], eG, pu], w=[S_gdn[h]])
                    sdst = S_gdn_bf[h][(si + c + 1) % 3]
                    P.op("act", lambda E, h=h, sdst=sdst: E.copy(out=sdst.t[:], in_=S_gdn[h].t[:]), r=[S_gdn[h]], w=[sdst])
            po_gdn = pbank()
            for h in range(4):
                hs = slice(h * 128, (h + 1) * 128)
                for c in range(2):
                    ssrc = S_gdn_bf[h][(si + c) % 3]
                    P.op("pe", lambda E, h=h, hs=hs, c=c, ssrc=ssrc: E.matmul(po_gdn.t[:, hs], lhsT=qdpad[h].t[:, 64 + c * 64:192 + c * 64], rhs=ssrc.t[:], start=(c == 0), stop=False), r=[qdpad[h], ssrc], w=[po_gdn])
                P.op("pe", lambda E, hs=hs: E.matmul(po_gdn.t[:, hs], lhsT=at_gdn.t[:, hs], rhs=vnew.t[:, hs], start=False, stop=True), r=[at_gdn, vnew], w=[po_gdn])

            DD("ogdn", po_gdn, 512, t0)
            norm_gate(1, po_gdn)
            if prev_route is not None:
                prev_route[0]()
            if CUT == 7:
                return
            tbm = tbank()
            for k in range(8):
                P.op("pe", lambda E, k=k, tbm=tbm: E.transpose(tbm.t[:, k * 128:(k + 1) * 128], mixed.t[:, k * 128:(k + 1) * 128], ident_bf.t[:]), r=[mixed, ident_bf], w=[tbm])
            mixT = tmp("mixT", [128, 1024], BF16, 1)
            P.op("act", lambda E, tbm=tbm: E.copy(out=mixT.t[:], in_=tbm.t[:]), r=[tbm], w=[mixT])
            z = tmp("z", [128, D], F32, 1)
            for half in range(2):
                py = pbank()
                for k in range(8):
                    P.op("pe", lambda E, k=k, py=py, half=half: E.matmul(py.t[:], lhsT=mixT.t[:, k * 128:(k + 1) * 128], rhs=wout_v[:, k, half * 512:(half + 1) * 512], start=(k == 0), stop=(k == 7)), r=[mixT, wout_c[k]], w=[py])
                P.op("dve", lambda E, py=py, half=half: E.scalar_tensor_tensor(out=z.t[:, half * 512:(half + 1) * 512], in0=xtok.t[:, half * 512:(half + 1) * 512], scalar=ALPHA, in1=py.t[:], op0=ALU.mult, op1=ALU.add), r=[xtok, py], w=[z])

            def layer_norm(z, g_ap, b_ap, gb_tl, out_tl):
                st = tmp("lnst", [128, 4], F32)
                junk = tmp("mixT", [128, 1024], BF16, 1)
                P.op("dve", lambda E: E.tensor_reduce(out=st.t[:, 0:1], in_=z.t[:], axis=AX.X, op=ALU.add), r=[z], w=[st])
                P.op("act", lambda E: E.activation(out=junk.t[:], in_=z.t[:], func=AF.Square, accum_out=st.t[:, 1:2]), r=[z], w=[junk, st])
                P.op("dve", lambda E: E.tensor_scalar(out=st.t[:, 0:2], in0=st.t[:, 0:2], scalar1=1.0 / D, scalar2=None, op0=ALU.mult), r=[st], w=[st])
                P.op("dve", lambda E: E.tensor_tensor(out=st.t[:, 2:3], in0=st.t[:, 0:1], in1=st.t[:, 0:1], op=ALU.mult), r=[st], w=[st])
                P.op("dve", lambda E: E.tensor_tensor(out=st.t[:, 2:3], in0=st.t[:, 1:2], in1=st.t[:, 2:3], op=ALU.subtract), r=[st], w=[st])
                P.op("act", lambda E: E.activation(out=st.t[:, 3:4], in_=st.t[:, 2:3], func=AF.Ln, bias=epsT.t[:, 1:2]), r=[st, epsT], w=[st])
                P.op("act", lambda E: E.activation(out=st.t[:, 3:4], in_=st.t[:, 3:4], func=AF.Exp, scale=-0.5), r=[st], w=[st])
                P.op("dve", lambda E: E.tensor_scalar(out=out_tl.t[:], in0=z.t[:], scalar1=st.t[:, 0:1], scalar2=st.t[:, 3:4], op0=ALU.subtract, op1=ALU.mult), r=[z, st], w=[out_tl])
                P.op("dve", lambda E: E.tensor_tensor(out=out_tl.t[:], in0=out_tl.t[:], in1=g_ap, op=ALU.mult), r=[out_tl, gb_tl], w=[out_tl])
                P.op("dve", lambda E: E.tensor_tensor(out=out_tl.t[:], in0=out_tl.t[:], in1=b_ap, op=ALU.add), r=[out_tl, gb_tl], w=[out_tl])

            x1 = z
            layer_norm(z, lnp.t[:, 0:D], lnp.t[:, D:2 * D], lnp, x1)
            if prev_route is not None:
                prev_route[1]()
            if dbg is True:
                P.dma("sp", lambda E, x1=x1, t0=t0: E.dma_start(out=dbg_d[t0:t0 + 128, :], in_=x1.t[:]), "dbg", r=[x1])
            if not moe:
                P.dma("sp", lambda E, x1=x1, t0=t0: E.dma_start(out=out_d[t0:t0 + 128, :], in_=x1.t[:]), "out", r=[x1])
                return
            P.dma("sp", lambda E: E.dma_start(out=x1f_d[t0:t0 + 128, :], in_=z.t[:]), "stx", r=[z, x1f_T], n=2)
            P.dma("pool", lambda E: E.dma_start(out=x1b_d[t0:t0 + 128, :], in_=z.t[:]), "stb", r=[z, x1b_T], n=2)
            rt = tmp("rt", [128, 160], F32, 2)
            M1 = tmp("M1", [128, 64], F32, 2)
            M2 = tmp("M2", [128, 64], F32, 2)
            T64 = tmp("T64", [128, 64], F32, 2)
            Msb = tmp("Msb", [128, 64], BF16, 2)
            it = tmp("it", [128, 8], I32, 2)
            iota8 = cst.t[:, C_IO:C_IO + 8]
            iota64 = cst.t[:, C_IO:C_IO + 64]

            def c(a, b=None):
                return rt.t[:, a:(b if b is not None else a + 1)]

            def V(fn, er=(), ew=()):
                P.op("dve", fn, r=[rt, *er], w=[rt, *ew])


            def router_pe():
                x1T = xtok
                for half in range(2):
                    pt = pbank()
                    for j in range(4):
                        k = half * 4 + j
                        P.op("pe", lambda E, pt=pt, j=j, k=k: E.transpose(pt.t[:, j * 128:(j + 1) * 128], z.t[:, k * 128:(k + 1) * 128], ident), r=[z, cst], w=[pt])
                    P.op("act", lambda E, pt=pt, half=half: E.copy(out=x1T.t[:, half * 512:(half + 1) * 512], in_=pt.t[:]), r=[pt], w=[x1T])
                plg = pbank()
                for k in range(8):
                    P.op("pe", lambda E, k=k: E.matmul(plg.t[:, 0:72], lhsT=x1T.t[:, k * 128:(k + 1) * 128], rhs=wr.t[:, k, :], start=(k == 0), stop=(k == 7)), r=[x1T, wr], w=[plg])
                V(lambda E: E.tensor_tensor(out=c(0, 72), in0=plg.t[:, 0:72], in1=small.t[:, P_RB:P_RB + 72], op=ALU.add), [plg, small])

            def route_a():
                V(lambda E: E.tensor_reduce(out=c(120), in_=c(0, 8), axis=AX.X, op=ALU.max))
                V(lambda E: E.tensor_scalar(out=c(72, 80), in0=c(0, 8), scalar1=c(120), scalar2=None, op0=ALU.is_equal))
                V(lambda E: E.tensor_scalar(out=c(121), in0=c(120), scalar1=-1.0, scalar2=None, op0=ALU.mult))
                P.op("act", lambda E: E.activation(out=c(140, 148), in_=c(0, 8), func=AF.Exp, bias=c(121)), r=[rt], w=[rt])
                V(lambda E: E.tensor_reduce(out=c(122), in_=c(140, 148), axis=AX.X, op=ALU.add))
                V(lambda E: E.reciprocal(out=c(123), in_=c(122)))
                V(lambda E: E.tensor_scalar(out=c(88, 96), in0=c(8, 16), scalar1=c(72), scalar2=None, op0=ALU.mult))
                for g in range(1, 8):
                    V(lambda E, g=g: E.scalar_tensor_tensor(out=c(88, 96), in0=c(8 + 8 * g, 16 + 8 * g), scalar=c(72 + g), in1=c(88, 96), op0=ALU.mult, op1=ALU.add))
                V(lambda E: E.tensor_reduce(out=c(124), in_=c(88, 96), axis=AX.X, op=ALU.max))
                V(lambda E: E.tensor_scalar(out=c(96, 104), in0=c(88, 96), scalar1=c(124), scalar2=None, op0=ALU.is_equal))
                V(lambda E: E.scalar_tensor_tensor(out=c(104, 112), in0=c(96, 104), scalar=-1e30, in1=c(88, 96), op0=ALU.mult, op1=ALU.add))
                V(lambda E: E.tensor_reduce(out=c(125), in_=c(104, 112), axis=AX.X, op=ALU.max))
                V(lambda E: E.tensor_scalar(out=c(112, 120), in0=c(104, 112), scalar1=c(125), scalar2=None, op0=ALU.is_equal))
                V(lambda E: E.tensor_tensor(out=c(126), in0=c(125), in1=c(124), op=ALU.subtract))
                P.op("act", lambda E: E.activation(out=c(126), in_=c(126), func=AF.Exp), r=[rt], w=[rt])
                V(lambda E: E.tensor_scalar(out=c(127), in0=c(126), scalar1=1.0, scalar2=None, op0=ALU.add))
                V(lambda E: E.reciprocal(out=c(127), in_=c(127)))
                V(lambda E: E.tensor_tensor(out=c(128), in0=c(126), in1=c(127), op=ALU.mult))
                P.op("dve", lambda E: E.tensor_tensor(out=gates.t[:, 2 * tt:2 * tt + 1], in0=c(123), in1=c(127), op=ALU.mult), r=[rt], w=[gates])
                P.op("dve", lambda E: E.tensor_tensor(out=gates.t[:, 2 * tt + 1:2 * tt + 2], in0=c(123), in1=c(128), op=ALU.mult), r=[rt], w=[gates])
                for src, dst in ((72, 129), (96, 130), (112, 131)):
                    V(lambda E, src=src: E.tensor_tensor(out=c(80, 88), in0=c(src, src + 8), in1=iota8, op=ALU.mult), [cst])
                    V(lambda E, dst=dst: E.tensor_reduce(out=c(dst), in_=c(80, 88), axis=AX.X, op=ALU.add))
                V(lambda E: E.scalar_tensor_tensor(out=c(132), in0=c(129), scalar=8.0, in1=c(130), op0=ALU.mult, op1=ALU.add))
                V(lambda E: E.scalar_tensor_tensor(out=c(133), in0=c(129), scalar=8.0, in1=c(131), op0=ALU.mult, op1=ALU.add))
                P.op("dve", lambda E: E.tensor_scalar(out=M1.t[:], in0=iota64, scalar1=c(132), scalar2=None, op0=ALU.is_equal), r=[rt, cst], w=[M1])
                P.op("dve", lambda E: E.tensor_scalar(out=M2.t[:], in0=iota64, scalar1=c(133), scalar2=None, op0=ALU.is_equal), r=[rt, cst], w=[M2])
                P.op("dve", lambda E: E.tensor_tensor(out=Msb.t[:], in0=M1.t[:], in1=M2.t[:], op=ALU.add), r=[M1, M2], w=[Msb])
            def route_b():
                pp = pbank()
                P.op("pe", lambda E: E.matmul(pp.t[:, 0:64], lhsT=u_bf.t[:], rhs=Msb.t[:], start=True, stop=False), r=[u_bf, Msb], w=[pp])
                P.op("pe", lambda E: E.matmul(pp.t[:, 0:64], lhsT=ones_bf.t[:], rhs=Macc.t[:], start=False, stop=True), r=[ones_bf, Macc], w=[pp])
                for Mx, dst in ((M1, 134), (M2, 135)):
                    P.op("dve", lambda E, Mx=Mx: E.tensor_tensor(out=T64.t[:], in0=Mx.t[:], in1=pp.t[:, 0:64], op=ALU.mult), r=[Mx, pp], w=[T64])
                    V(lambda E, dst=dst: E.tensor_reduce(out=c(dst), in_=T64.t[:], axis=AX.X, op=ALU.add), [T64])
                P.op("dve", lambda E: E.tensor_tensor(out=Macc.t[:], in0=Macc.t[:], in1=Msb.t[:], op=ALU.add), r=[Macc, Msb], w=[Macc])
                for eid, sl, dst, ov in ((132, 134, 136, 141), (133, 135, 137, 142)):
                    V(lambda E, eid=eid, sl=sl, dst=dst: E.scalar_tensor_tensor(out=c(dst), in0=c(eid), scalar=float(CAP), in1=c(sl), op0=ALU.mult, op1=ALU.add))
                    V(lambda E, sl=sl, ov=ov: E.tensor_scalar(out=c(ov), in0=c(sl), scalar1=CAP - 0.5, scalar2=None, op0=ALU.is_gt))
                    V(lambda E, dst=dst, ov=ov: E.scalar_tensor_tensor(out=c(ov), in0=c(dst), scalar=-float(NEXP * CAP), in1=c(ov), op0=ALU.add, op1=ALU.mult))
                    V(lambda E, dst=dst, ov=ov: E.tensor_tensor(out=c(dst), in0=c(dst), in1=c(ov), op=ALU.subtract))
                V(lambda E: E.tensor_scalar(out=c(138), in0=cst.t[:, C_TOK:C_TOK + 1], scalar1=float(t0), scalar2=None, op0=ALU.add), [cst])
                V(lambda E: E.tensor_scalar(out=c(139), in0=c(138), scalar1=float(YP), scalar2=None, op0=ALU.add))
                P.op("dve", lambda E: E.tensor_copy(out=it.t[:, 0:2], in_=c(136, 138)), r=[rt], w=[it])
                P.op("dve", lambda E: E.tensor_copy(out=dest_i.t[:, 2 * tt:2 * tt + 2], in_=c(136, 138)), r=[rt], w=[dest_i])
                for col, src in ((2, 138), (3, 138), (4, 138), (5, 139)):
                    P.op("dve", lambda E, col=col, src=src: E.tensor_copy(out=it.t[:, col:col + 1], in_=c(src)), r=[rt], w=[it])
                for k in range(2):
                    P.dma("pool", lambda E, k=k: E.indirect_dma_start(out=tab_d, out_offset=bass.IndirectOffsetOnAxis(ap=it.t[:, k:k + 1], axis=0), in_=it.t[:, 2 + 2 * k:4 + 2 * k], in_offset=None), "tabs", r=[it], w=[tab_T], n=2)
                DD("rt", rt, 160, t0)

            return (route_a, route_b, router_pe)

        pending = None
        if nt > 0:
            load_xT(0)
        for tt in range(nt):
            pending = tile_body(tt, pending)
        if pending is not None:
            pending[2]()
            pending[0]()
            pending[1]()

        if moe:
            idx_sb = sb([128, NEXP, 2], I32, "idx_sb")
            P.dma("sp", lambda E: E.dma_start(out=idx_sb.t[:], in_=tab_d[0:NEXP * CAP, :].rearrange("(e s) c -> s e c", s=CAP)), "idx", w=[idx_sb, tab_T])
            wslots = [[DT() for _ in range(3)] for _ in range(3)]
            WSZ = 12288

            def load_w(e):
                sl = e % 3
                base = sl * WSZ
                first = e < 3
                ws = wslots[sl]
                wl = [((win_c + wout_c + [ws[m]]) if first else [ws[m]]) for m in range(3)]
                gv = big.t[:, base:base + 4096].rearrange("p (k f) -> p k f", k=8)
                uv = big.t[:, base + 4096:base + 8192].rearrange("p (k f) -> p k f", k=8)
                dv = big.t[:, base + 8192:base + 12288].rearrange("p (k f) -> p k f", k=4)
                P.dma("pool", lambda E: E.dma_start(out=gv, in_=wg_d[e].rearrange("(k p) f -> p k f", p=128)), "wexp", w=wl[0], n=9)
                P.dma("pool", lambda E: E.dma_start(out=uv, in_=wu_d[e].rearrange("(k p) f -> p k f", p=128)), "wexp", w=wl[1], n=9)
                P.dma("pool", lambda E: E.dma_start(out=dv, in_=wd_d[e].rearrange("(k p) f -> p k f", p=128)), "wexp", w=wl[2], n=9)
                return gv, uv, dv, ws

            class View2:
                def __init__(self, tl):
                    self.t = tl.t[:].rearrange("p k t -> p (k t)")
                    self.b = tl.b
            xg_bufs = [View2(tmp("xTt", [128, 8, 128], BF16, 2)), View2(tmp("xTt", [128, 8, 128], BF16, 2))]

            def gather(e):
                xg = xg_bufs[e % 2]
                P.dma("pool", lambda E: E.indirect_dma_start(out=xg.t[:], out_offset=None, in_=x1b_d, in_offset=bass.IndirectOffsetOnAxis(ap=idx_sb.t[:, e, 0:1], axis=0)), "gath", r=([idx_sb] if e == 0 else [idx_sb, x1b_T]), w=([xg, x1b_T] if e == 0 else [xg]), n=2)
                return xg

            def expert(e, wv, xg):
                gv, uv, dv, ws = wv
                tb = tbank()
                for k in range(8):
                    P.op("pe", lambda E, k=k: E.transpose(tb.t[:, k * 128:(k + 1) * 128], xg.t[:, k * 128:(k + 1) * 128], ident_bf.t[:]), r=[xg, ident_bf], w=[tb])
                xgT = tmp("mixT", [128, 1024], BF16, 1)
                P.op("act", lambda E: E.copy(out=xgT.t[:], in_=tb.t[:]), r=[tb], w=[xgT])
                ph = []
                for mi_, wv_ in enumerate((gv, uv)):
                    pb = pbank()
                    for k in range(8):
                        P.op("pe", lambda E, k=k, pb=pb, wv_=wv_: E.matmul(pb.t[:], lhsT=xgT.t[:, k * 128:(k + 1) * 128], rhs=wv_[:, k, :], start=(k == 0), stop=(k == 7)), r=[xgT, ws[mi_]], w=[pb])
                    ph.append(pb)
                sg = tmp("sq2", [128, 512], F32, 1)
                P.op("act", lambda E: E.activation(out=sg.t[:], in_=ph[0].t[:], func=AF.Silu), r=[ph[0]], w=[sg])
                hid = tmp("at_gla", [128, 512], BF16)
                P.op("dve", lambda E: E.tensor_tensor(out=hid.t[:], in0=sg.t[:], in1=ph[1].t[:], op=ALU.mult), r=[sg, ph[1]], w=[hid])
                tb2 = tbank()
                for f in range(4):
                    P.op("pe", lambda E, f=f: E.transpose(tb2.t[:, f * 128:(f + 1) * 128], hid.t[:, f * 128:(f + 1) * 128], ident_bf.t[:]), r=[hid, ident_bf], w=[tb2])
                hidT = tmp("at_gdn", [128, 512], BF16)
                P.op("act", lambda E: E.copy(out=hidT.t[:], in_=tb2.t[:, 0:512]), r=[tb2], w=[hidT])
                ysb = tmp("z", [128, D], F32, 1) if e % 2 == 0 else tmp("xtok", [128, D], F32, 1)
                for half in range(2):
                    py = pbank()
                    for f in range(4):
                        P.op("pe", lambda E, f=f, py=py, half=half: E.matmul(py.t[:], lhsT=hidT.t[:, f * 128:(f + 1) * 128], rhs=dv[:, f, half * 512:(half + 1) * 512], start=(f == 0), stop=(f == 3)), r=[hidT, ws[2]], w=[py])
                    eng = "act" if half == 0 else "dve"
                    if eng == "act":
                        P.op("act", lambda E, py=py, half=half: E.copy(out=ysb.t[:, half * 512:(half + 1) * 512], in_=py.t[:]), r=[py], w=[ysb])
                    else:
                        P.op("dve", lambda E, py=py, half=half: E.tensor_copy(out=ysb.t[:, half * 512:(half + 1) * 512], in_=py.t[:]), r=[py], w=[ysb])
                P.dma("sp", lambda E: E.dma_start(out=ysc_d[e * CAP:(e + 1) * CAP, :], in_=ysb.t[:]), "yst", r=[ysb, ysc_T], n=2)

            ne = NEXP
            wq = {0: load_w(0)}
            if ne > 1:
                wq[1] = load_w(1)
            xgs = {0: gather(0)}
            for e in range(ne):
                if e + 1 < ne:
                    xgs[e + 1] = gather(e + 1)
                if e + 2 < ne:
                    wq[e + 2] = load_w(e + 2)
                expert(e, wq.pop(e), xgs.pop(e))

            P.dma("sp", lambda E: E.dma_start(out=lnp.t[:], in_=ln2_d), "ldd", w=[lnp])

            def layer_norm2(z, g_ap, b_ap, gb_tl):
                st = tmp("lnst", [128, 4], F32)
                junk = tmp("mixT", [128, 1024], BF16, 1)
                P.op("dve", lambda E: E.tensor_reduce(out=st.t[:, 0:1], in_=z.t[:], axis=AX.X, op=ALU.add), r=[z], w=[st])
                P.op("act", lambda E: E.activation(out=junk.t[:], in_=z.t[:], func=AF.Square, accum_out=st.t[:, 1:2]), r=[z], w=[junk, st])
                P.op("dve", lambda E: E.tensor_scalar(out=st.t[:, 0:2], in0=st.t[:, 0:2], scalar1=1.0 / D, scalar2=None, op0=ALU.mult), r=[st], w=[st])
                P.op("dve", lambda E: E.tensor_tensor(out=st.t[:, 2:3], in0=st.t[:, 0:1], in1=st.t[:, 0:1], op=ALU.mult), r=[st], w=[st])
                P.op("dve", lambda E: E.tensor_tensor(out=st.t[:, 2:3], in0=st.t[:, 1:2], in1=st.t[:, 2:3], op=ALU.subtract), r=[st], w=[st])
                P.op("act", lambda E: E.activation(out=st.t[:, 3:4], in_=st.t[:, 2:3], func=AF.Ln, bias=epsT.t[:, 1:2]), r=[st, epsT], w=[st])
                P.op("act", lambda E: E.activation(out=st.t[:, 3:4], in_=st.t[:, 3:4], func=AF.Exp, scale=-0.5), r=[st], w=[st])
                P.op("dve", lambda E: E.tensor_scalar(out=z.t[:], in0=z.t[:], scalar1=st.t[:, 0:1], scalar2=st.t[:, 3:4], op0=ALU.subtract, op1=ALU.mult), r=[z, st], w=[z])
                P.op("dve", lambda E: E.tensor_tensor(out=z.t[:], in0=z.t[:], in1=g_ap, op=ALU.mult), r=[z, gb_tl], w=[z])
                P.op("dve", lambda E: E.tensor_tensor(out=z.t[:], in0=z.t[:], in1=b_ap, op=ALU.add), r=[z, gb_tl], w=[z])

            class View:
                def __init__(self, ap):
                    self.t = ap
                    self.b = Buf()
            all_ws = [w_ for sl_ in wslots for w_ in sl_]
            f3 = [[View(big.t[:, (3 * i + j) * 2048:(3 * i + j + 1) * 2048].bitcast(F32)) for j in range(3)] for i in range(3)]
            f3_first = set()

            def final_load(tt):
                t0 = tt * 128
                acc, y0, y1 = f3[tt % 3]
                extra = []
                if tt % 3 not in f3_first and tt < 3:
                    extra = all_ws
                    f3_first.add(tt % 3)
                P.dma("sp", lambda E: E.dma_start(out=acc.t[:], in_=x1f_d[t0:t0 + 128, :]), "f0", r=([] if tt == 0 else [x1f_T]), w=([acc, x1f_T] if tt == 0 else [acc]) + extra, n=2)
                P.dma("pool", lambda E: E.indirect_dma_start(out=y0.t[:], out_offset=None, in_=ysc_d, in_offset=bass.IndirectOffsetOnAxis(ap=dest_i.t[:, 2 * tt:2 * tt + 1], axis=0)), "f1", r=([dest_i] if tt == 0 else [dest_i, ysc_T]), w=([y0, ysc_T] if tt == 0 else [y0]) + extra, n=2)
                P.dma("pool", lambda E: E.indirect_dma_start(out=y1.t[:], out_offset=None, in_=ysc_d, in_offset=bass.IndirectOffsetOnAxis(ap=dest_i.t[:, 2 * tt + 1:2 * tt + 2], axis=0)), "f2", r=[dest_i, ysc_T], w=[y1] + extra, n=2)

            def final_tile(tt):
                t0 = tt * 128
                acc, y0, y1 = f3[tt % 3]
                P.op("dve", lambda E: E.scalar_tensor_tensor(out=y0.t[:], in0=y0.t[:], scalar=gates.t[:, 2 * tt:2 * tt + 1], in1=y1.t[:], op0=ALU.mult, op1=ALU.bypass) if False else E.tensor_scalar(out=y0.t[:], in0=y0.t[:], scalar1=gates.t[:, 2 * tt:2 * tt + 1], scalar2=None, op0=ALU.mult), r=[y0, gates], w=[y0])
                P.op("dve", lambda E: E.scalar_tensor_tensor(out=y0.t[:], in0=y1.t[:], scalar=gates.t[:, 2 * tt + 1:2 * tt + 2], in1=y0.t[:], op0=ALU.mult, op1=ALU.add), r=[y1, gates, y0], w=[y0])
                P.op("dve", lambda E: E.scalar_tensor_tensor(out=acc.t[:], in0=acc.t[:], scalar=ALPHA, in1=y0.t[:], op0=ALU.mult, op1=ALU.add), r=[acc, y0], w=[acc])
                layer_norm2(acc, lnp.t[:, 0:D], lnp.t[:, D:2 * D], lnp)
                P.dma("sp", lambda E: E.dma_start(out=out_d[t0:t0 + 128, :], in_=acc.t[:]), "out", r=[acc], n=2)

            for tt in range(min(2, nt)):
                final_load(tt)
            for tt in range(nt):
                if tt + 2 < nt:
                    final_load(tt + 2)
                final_tile(tt)

        for key in list(P.dcount):
            P.wait_all_dma("sp", key.rstrip("0123456789"))
        P.emit()
    return nc


def prep_inputs(inputs):
    f = np.float32
    x = np.asarray(inputs["x"], f)
    small = np.zeros((128, PW), f)
    small[:, P_BGK:P_BGK + 2] = np.asarray(inputs["b_gk"][0], f).reshape(2, 128).T
    small[:, P_CONV:P_CONV + 48] = np.asarray(inputs["conv_w"][0], f).T.reshape(12, 128, 4).transpose(1, 0, 2).reshape(128, 48)
    small[:, P_ALOG:P_ALOG + 4] = np.asarray(inputs["a_log"][0], f)[None, :]
    small[:, P_DTB:P_DTB + 4] = np.asarray(inputs["dt_bias"][0], f)[None, :]
    small[:, P_RB:P_RB + 8] = np.asarray(inputs["b_router_group"][0], f)[None, :]
    small[:, P_RB + 8:P_RB + 72] = np.asarray(inputs["b_router_expert"][0], f)[None, :]
    gn = np.concatenate([np.tile(np.asarray(inputs["gla_norm_g"][0], f), 4), np.tile(np.asarray(inputs["gdn_norm_g"][0], f), 4)])
    gn = np.ascontiguousarray(np.broadcast_to(gn[None, :], (128, 1024)))
    ln1 = np.ascontiguousarray(np.broadcast_to(np.concatenate([inputs["ln1_g"][0], inputs["ln1_b"][0]]).astype(f)[None, :], (128, 2048)))
    ln2 = np.ascontiguousarray(np.broadcast_to(np.concatenate([inputs["ln2_g"][0], inputs["ln2_b"][0]]).astype(f)[None, :], (128, 2048)))
    w_r = np.ascontiguousarray(np.concatenate([inputs["w_router_group"][0], inputs["w_router_expert"][0]], axis=1).astype(f))
    shared = {
        "w_in": np.ascontiguousarray(inputs["w_in"][0], f),
        "w_out": np.ascontiguousarray(inputs["w_out"][0], f),
        "w_gk": np.ascontiguousarray(inputs["w_gk_up"][0], f),
        "small": small, "gn": gn, "ln1": ln1, "ln2": ln2, "consts": make_consts(), "w_r": w_r,
        "w_gate": np.ascontiguousarray(inputs["w_gate"][0], f),
        "w_up": np.ascontiguousarray(inputs["w_up"][0], f),
        "w_down": np.ascontiguousarray(inputs["w_down"][0], f),
    }
    maps = []
    for b in range(NCORES):
        m = dict(shared)
        m["x"] = np.ascontiguousarray(x[b])
        m["xT"] = np.ascontiguousarray(x[b].T)
        maps.append(m)
    return maps


def kernel(**inputs):
    maps = prep_inputs(inputs)
    nc = build()
    res = run_bass_kernel_spmd(nc, maps, core_ids=list(range(NCORES)))
    return np.stack([np.asarray(r["out"], np.float32) for r in res.results], axis=0)
```
